# Optimizing a Trainium2 kernel written in Bass

```python
import math
import jax
import jax.numpy as jnp
from jax import lax
import numpy as np


D_MODEL = 1024
BATCH = 16
SEQ = 2048
DEPTH = 2

MEM_LEN = 256
CHUNK = 128
SGU_GROUPS = 4
SGU_GROUP_DIM = 128
SGU_WIDTH = SGU_GROUPS * SGU_GROUP_DIM
DIFF_HEADS = 4
DIFF_HEAD_DIM = 64
DIFF_V_DIM = 2 * DIFF_HEAD_DIM
DIFF_QK_WIDTH = DIFF_HEADS * 2 * DIFF_HEAD_DIM
DIFF_V_WIDTH = DIFF_HEADS * DIFF_V_DIM
Q_BLOCK = 128
IN_EVEN = 2 * SGU_WIDTH + 2 * DIFF_QK_WIDTH + DIFF_V_WIDTH
MIX_EVEN = SGU_WIDTH + DIFF_V_WIDTH
S5_WIDTH = D_MODEL
S5_GROUP = 16
S5_GROUPS = S5_WIDTH // S5_GROUP
S5_STATE = 64
DT_MIN = 0.001
DT_MAX = 0.1
X_HEADS = 4
X_HEAD_DIM = D_MODEL // X_HEADS
N_EXPERTS = 32
TOP_K = 4
D_FF = D_MODEL
SWIGLU_LIMIT = 7.0
SWIGLU_ALPHA = 1.702
MOE_BLOCK = 256
ALPHA = (2 * DEPTH) ** 0.25
BETA = (8 * DEPTH) ** -0.25
LN_EPS = 1e-5
NEG_INF = -1e30

kernel_name = 'hybrid_sgu_diffattn_s5_moe_block'


def layer_norm(x, g, b):
    xf = x.astype(jnp.float32)
    mu = jnp.mean(xf, axis=-1, keepdims=True)
    var = jnp.mean(jnp.square(xf - mu), axis=-1, keepdims=True)
    return ((xf - mu) * lax.rsqrt(var + LN_EPS)).astype(x.dtype) * g + b


def rms_norm(x, g):
    xf = x.astype(jnp.float32)
    return (xf * lax.rsqrt(jnp.mean(jnp.square(xf), axis=-1, keepdims=True) + LN_EPS)).astype(x.dtype) * g


def diff_lambda_init(layer_idx):
    return 0.8 - 0.6 * math.exp(-0.3 * layer_idx)


def spatial_gating(u, v, ln_g, ln_b, w_spatial, b_spatial):
    b, s, _ = u.shape
    u = jax.nn.gelu(u)
    v = jax.nn.gelu(v).reshape(b, s, SGU_GROUPS, SGU_GROUP_DIM)
    v = layer_norm(v, ln_g.reshape(SGU_GROUPS, SGU_GROUP_DIM), ln_b.reshape(SGU_GROUPS, SGU_GROUP_DIM))
    v = v.reshape(b, s // CHUNK, CHUNK, SGU_GROUPS, SGU_GROUP_DIM)
    w = jnp.tril(w_spatial)
    gate = jnp.einsum('gts,bcsgd->bctgd', w, v) + b_spatial.T[None, None, :, :, None]
    return u * gate.reshape(b, s, SGU_WIDTH)


def diff_attention(q, k, v, lam_q1, lam_k1, lam_q2, lam_k2, subln_g, lam_init):
    b, s, _ = q.shape
    q = q.reshape(b, s, DIFF_HEADS, 2, DIFF_HEAD_DIM)
    k = k.reshape(b, s, DIFF_HEADS, 2, DIFF_HEAD_DIM)
    v = v.reshape(b, s, DIFF_HEADS, DIFF_V_DIM)
    lam = (jnp.exp(jnp.sum(lam_q1 * lam_k1).astype(jnp.float32))
           - jnp.exp(jnp.sum(lam_q2 * lam_k2).astype(jnp.float32)) + lam_init)
    n_blk = s // Q_BLOCK
    q_blocks = jnp.moveaxis(q.reshape(b, n_blk, Q_BLOCK, DIFF_HEADS, 2, DIFF_HEAD_DIM), 1, 0)
    k_pos = jnp.arange(s)
    scale = DIFF_HEAD_DIM ** -0.5

    def attend_block(args):
        qb, blk = args
        q_pos = blk * Q_BLOCK + jnp.arange(Q_BLOCK)
        mask = k_pos[None, :] <= q_pos[:, None]
        sc = jnp.einsum('bqhcd,bkhcd->bhcqk', qb, k).astype(jnp.float32) * scale
        p = jax.nn.softmax(jnp.where(mask, sc, NEG_INF), axis=-1)
        attn = p[:, :, 0] - lam * p[:, :, 1]
        return jnp.einsum('bhqk,bkhe->bqhe', attn.astype(v.dtype), v)

    o = lax.map(attend_block, (q_blocks, jnp.arange(n_blk)))
    o = jnp.moveaxis(o, 0, 1).reshape(b, s, DIFF_HEADS, DIFF_V_DIM)
    o = rms_norm(o, subln_g) * (1.0 - lam_init)
    return o.reshape(b, s, DIFF_V_WIDTH)


def even_mixer(x, w_in, sgu_ln_g, sgu_ln_b, w_spatial, b_spatial,
               lam_q1, lam_k1, lam_q2, lam_k2, subln_g, w_out, lam_init):
    h = x @ w_in
    u, v_s, q, k, v_d = jnp.split(
        h, [SGU_WIDTH, 2 * SGU_WIDTH, 2 * SGU_WIDTH + DIFF_QK_WIDTH,
            2 * SGU_WIDTH + 2 * DIFF_QK_WIDTH], axis=-1)
    a = spatial_gating(u, v_s, sgu_ln_g, sgu_ln_b, w_spatial, b_spatial)
    d = diff_attention(q, k, v_d, lam_q1, lam_k1, lam_q2, lam_k2, subln_g, lam_init)
    return jnp.concatenate([a, d], axis=-1) @ w_out


def s5_mixer(x, w_in, log_dt, lambda_re, lambda_im, b_re, b_im, c_re, c_im, d_skip, w_val, w_gate):
    b, s, _ = x.shape
    u = (x @ w_in).reshape(b, s, S5_GROUPS, S5_GROUP).astype(jnp.float32)
    dt = jnp.exp(log_dt.astype(jnp.float32))[:, None]
    lr = lambda_re.astype(jnp.float32)
    li = lambda_im.astype(jnp.float32)
    mag = jnp.exp(lr * dt)
    ar = mag * jnp.cos(li * dt)
    ai = mag * jnp.sin(li * dt)
    den = lr * lr + li * li
    zr = ((ar - 1.0) * lr + ai * li) / den
    zi = (ai * lr - (ar - 1.0) * li) / den
    br = b_re.astype(jnp.float32)
    bi = b_im.astype(jnp.float32)
    bbar_re = zr[..., None] * br - zi[..., None] * bi
    bbar_im = zr[..., None] * bi + zi[..., None] * br
    bu_re = jnp.einsum('bsgc,gpc->bsgp', u, bbar_re)
    bu_im = jnp.einsum('bsgc,gpc->bsgp', u, bbar_im)
    a_re = jnp.broadcast_to(ar, (1, s, S5_GROUPS, S5_STATE))
    a_im = jnp.broadcast_to(ai, (1, s, S5_GROUPS, S5_STATE))

    def combine(e1, e2):
        a1r, a1i, b1r, b1i = e1
        a2r, a2i, b2r, b2i = e2
        return (a2r * a1r - a2i * a1i,
                a2r * a1i + a2i * a1r,
                a2r * b1r - a2i * b1i + b2r,
                a2r * b1i + a2i * b1r + b2i)

    _, _, xr, xi = lax.associative_scan(combine, (a_re, a_im, bu_re, bu_im), axis=1)
    y = (jnp.einsum('bsgp,gcp->bsgc', xr, c_re.astype(jnp.float32))
         - jnp.einsum('bsgp,gcp->bsgc', xi, c_im.astype(jnp.float32))
         + d_skip.astype(jnp.float32) * u)
    y = jax.nn.gelu(y.reshape(b, s, S5_WIDTH)).astype(x.dtype)
    return (y @ w_val) * jax.nn.sigmoid(y @ w_gate)


def memory_cross_attention(x, k_mem, v_mem, w_q, w_o):
    b, s, _ = x.shape
    q = (x @ w_q).reshape(b, s, X_HEADS, X_HEAD_DIM)
    sc = jnp.einsum('bshd,bmhd->bhsm', q, k_mem).astype(jnp.float32) * (X_HEAD_DIM ** -0.5)
    p = jax.nn.softmax(sc, axis=-1)
    o = jnp.einsum('bhsm,bmhd->bshd', p.astype(v_mem.dtype), v_mem).reshape(b, s, D_MODEL)
    return o @ w_o


def moe_ffn(x, router_w, router_b, w_up, b_up, w_down, b_down):
    b, s, d = x.shape
    n_tok = b * s
    x2d = x.reshape(n_tok, d)
    logits = (x2d @ router_w + router_b).astype(jnp.float32)
    top_logit, top_idx = lax.top_k(logits, TOP_K)
    gates = jax.nn.softmax(top_logit, axis=-1).astype(x.dtype)
    n_assign = n_tok * TOP_K
    flat_e = top_idx.reshape(n_assign)
    flat_t = jnp.repeat(jnp.arange(n_tok, dtype=jnp.int32), TOP_K)
    flat_g = gates.reshape(n_assign)
    order = jnp.argsort(flat_e)
    se, st, sg = flat_e[order], flat_t[order], flat_g[order]
    counts = jnp.bincount(flat_e, length=N_EXPERTS)
    start = jnp.cumsum(counts) - counts
    padded = (counts + MOE_BLOCK - 1) // MOE_BLOCK * MOE_BLOCK
    pend = jnp.cumsum(padded)
    pstart = pend - padded
    dest = pstart[se] + (jnp.arange(n_assign) - start[se])
    n_blocks = -(-n_assign // MOE_BLOCK) + N_EXPERTS
    n_pad = n_blocks * MOE_BLOCK
    buf_tok = jnp.zeros((n_pad,), jnp.int32).at[dest].set(st)
    buf_gate = jnp.zeros((n_pad,), x.dtype).at[dest].set(sg)
    blk_expert = jnp.minimum(
        jnp.searchsorted(pend, jnp.arange(n_blocks) * MOE_BLOCK, side='right'), N_EXPERTS - 1)

    def expert_block(args):
        tok, gate, e = args
        h = x2d[tok] @ w_up[e] + b_up[e]
        h_glu, h_lin = jnp.split(h, 2, axis=-1)
        h_glu = jnp.minimum(h_glu, SWIGLU_LIMIT)
        h_lin = jnp.clip(h_lin, -SWIGLU_LIMIT, SWIGLU_LIMIT)
        act = h_glu * jax.nn.sigmoid(SWIGLU_ALPHA * h_glu) * (h_lin + 1.0)
        return (act @ w_down[e] + b_down[e]) * gate[:, None]

    ys = lax.map(expert_block, (buf_tok.reshape(n_blocks, MOE_BLOCK),
                                buf_gate.reshape(n_blocks, MOE_BLOCK), blk_expert))
    y = jnp.zeros_like(x2d).at[buf_tok].add(ys.reshape(n_pad, d))
    return y.reshape(b, s, d)


def setup_inputs(seed: int = 0) -> dict:
    keys = iter(jax.random.split(jax.random.key(seed), 128))

    def nrm(shape, scale):
        return scale * jax.random.normal(next(keys), shape, jnp.float32)

    def gain(shape):
        return 1.0 + nrm(shape, 0.02)

    def small(shape):
        return nrm(shape, 0.01)

    fan = D_MODEL ** -0.5
    p = {}
    p['x'] = nrm((BATCH, SEQ, D_MODEL), 1.0)
    p['mem'] = nrm((BATCH, MEM_LEN, D_MODEL), 1.0)
    p['w_mem_kv'] = nrm((D_MODEL, 2 * D_MODEL), fan)

    def common(pre):
        p[pre + 'ln1_g'] = gain((D_MODEL,))
        p[pre + 'ln1_b'] = small((D_MODEL,))
        p[pre + 'xq'] = nrm((D_MODEL, D_MODEL), fan)
        p[pre + 'xo'] = nrm((D_MODEL, D_MODEL), BETA * fan)
        p[pre + 'ln2_g'] = gain((D_MODEL,))
        p[pre + 'ln2_b'] = small((D_MODEL,))
        p[pre + 'router_w'] = nrm((D_MODEL, N_EXPERTS), fan)
        p[pre + 'router_b'] = small((N_EXPERTS,))
        p[pre + 'exp_w_up'] = nrm((N_EXPERTS, D_MODEL, 2 * D_FF), fan)
        p[pre + 'exp_b_up'] = small((N_EXPERTS, 2 * D_FF))
        p[pre + 'exp_w_down'] = nrm((N_EXPERTS, D_FF, D_MODEL), BETA * D_FF ** -0.5)
        p[pre + 'exp_b_down'] = small((N_EXPERTS, D_MODEL))
        p[pre + 'ln3_g'] = gain((D_MODEL,))
        p[pre + 'ln3_b'] = small((D_MODEL,))

    p['l0_w_in'] = nrm((D_MODEL, IN_EVEN), fan)
    p['l0_sgu_ln_g'] = gain((SGU_WIDTH,))
    p['l0_sgu_ln_b'] = small((SGU_WIDTH,))
    p['l0_w_spatial'] = nrm((SGU_GROUPS, CHUNK, CHUNK), 0.5 * CHUNK ** -0.5)
    p['l0_b_spatial'] = gain((SGU_GROUPS, CHUNK))
    p['l0_lam_q1'] = nrm((DIFF_HEAD_DIM,), 0.1)
    p['l0_lam_k1'] = nrm((DIFF_HEAD_DIM,), 0.1)
    p['l0_lam_q2'] = nrm((DIFF_HEAD_DIM,), 0.1)
    p['l0_lam_k2'] = nrm((DIFF_HEAD_DIM,), 0.1)
    p['l0_subln_g'] = gain((DIFF_V_DIM,))
    p['l0_w_out'] = nrm((MIX_EVEN, D_MODEL), BETA * MIX_EVEN ** -0.5)
    common('l0_')
    p['l1_w_in'] = nrm((D_MODEL, S5_WIDTH), fan)
    p['l1_log_dt'] = jax.random.uniform(next(keys), (S5_GROUPS,), jnp.float32,
                                        minval=math.log(DT_MIN), maxval=math.log(DT_MAX))
    p['l1_lambda_re'] = -0.5 + small((S5_GROUPS, S5_STATE))
    p['l1_lambda_im'] = (math.pi * jnp.arange(S5_STATE, dtype=jnp.float32))[None, :] + small((S5_GROUPS, S5_STATE))
    p['l1_b_re'] = nrm((S5_GROUPS, S5_STATE, S5_GROUP), (2 * S5_GROUP) ** -0.5)
    p['l1_b_im'] = nrm((S5_GROUPS, S5_STATE, S5_GROUP), (2 * S5_GROUP) ** -0.5)
    p['l1_c_re'] = nrm((S5_GROUPS, S5_GROUP, S5_STATE), 0.5)
    p['l1_c_im'] = nrm((S5_GROUPS, S5_GROUP, S5_STATE), 0.5)
    p['l1_d_skip'] = nrm((S5_GROUPS, S5_GROUP), 1.0)
    p['l1_w_val'] = nrm((S5_WIDTH, D_MODEL), BETA * S5_WIDTH ** -0.5)
    p['l1_w_gate'] = nrm((S5_WIDTH, D_MODEL), S5_WIDTH ** -0.5)
    common('l1_')
    return p


def reference(x, mem, w_mem_kv,
              l0_w_in, l0_sgu_ln_g, l0_sgu_ln_b, l0_w_spatial, l0_b_spatial,
              l0_lam_q1, l0_lam_k1, l0_lam_q2, l0_lam_k2, l0_subln_g, l0_w_out,
              l0_ln1_g, l0_ln1_b, l0_xq, l0_xo, l0_ln2_g, l0_ln2_b,
              l0_router_w, l0_router_b, l0_exp_w_up, l0_exp_b_up, l0_exp_w_down, l0_exp_b_down,
              l0_ln3_g, l0_ln3_b,
              l1_w_in, l1_log_dt, l1_lambda_re, l1_lambda_im, l1_b_re, l1_b_im, l1_c_re, l1_c_im,
              l1_d_skip, l1_w_val, l1_w_gate,
              l1_ln1_g, l1_ln1_b, l1_xq, l1_xo, l1_ln2_g, l1_ln2_b,
              l1_router_w, l1_router_b, l1_exp_w_up, l1_exp_b_up, l1_exp_w_down, l1_exp_b_down,
              l1_ln3_g, l1_ln3_b):
    b = mem.shape[0]
    kv = mem @ w_mem_kv
    k_mem = kv[..., :D_MODEL].reshape(b, MEM_LEN, X_HEADS, X_HEAD_DIM)
    v_mem = kv[..., D_MODEL:].reshape(b, MEM_LEN, X_HEADS, X_HEAD_DIM)

    layers = [
        dict(mixer=(l0_w_in, l0_sgu_ln_g, l0_sgu_ln_b, l0_w_spatial, l0_b_spatial,
                    l0_lam_q1, l0_lam_k1, l0_lam_q2, l0_lam_k2, l0_subln_g, l0_w_out),
             ln1=(l0_ln1_g, l0_ln1_b), cross=(l0_xq, l0_xo), ln2=(l0_ln2_g, l0_ln2_b),
             moe=(l0_router_w, l0_router_b, l0_exp_w_up, l0_exp_b_up, l0_exp_w_down, l0_exp_b_down),
             ln3=(l0_ln3_g, l0_ln3_b)),
        dict(mixer=(l1_w_in, l1_log_dt, l1_lambda_re, l1_lambda_im, l1_b_re, l1_b_im,
                    l1_c_re, l1_c_im, l1_d_skip, l1_w_val, l1_w_gate),
             ln1=(l1_ln1_g, l1_ln1_b), cross=(l1_xq, l1_xo), ln2=(l1_ln2_g, l1_ln2_b),
             moe=(l1_router_w, l1_router_b, l1_exp_w_up, l1_exp_b_up, l1_exp_w_down, l1_exp_b_down),
             ln3=(l1_ln3_g, l1_ln3_b)),
    ]
    for i in range(DEPTH):
        prm = layers[i]
        if i % 2 == 0:
            h = even_mixer(x, *prm['mixer'], lam_init=diff_lambda_init(i))
        else:
            h = s5_mixer(x, *prm['mixer'])
        x = layer_norm(ALPHA * x + h, *prm['ln1'])
        x = layer_norm(ALPHA * x + memory_cross_attention(x, k_mem, v_mem, *prm['cross']), *prm['ln2'])
        x = layer_norm(ALPHA * x + moe_ffn(x, *prm['moe']), *prm['ln3'])
    return x
```

```python
import math
import numpy as np
from contextlib import ExitStack
import concourse.bass as bass
import concourse.mybir as mybir
from concourse.bass_utils import run_bass_kernel_spmd

F32 = mybir.dt.float32
BF16 = mybir.dt.bfloat16
I32 = mybir.dt.int32
ALU = mybir.AluOpType
AF = mybir.ActivationFunctionType
AX = mybir.AxisListType

SEM_ROLL = 24000
NDS = 10

NCORES = 8
NTOK = 4096
SEQ = 2048
D = 1024
ALPHA = 4.0 ** 0.25
EPS = 1e-5
LAM_INIT = 0.8 - 0.6 * math.exp(0.0)
PI = math.pi
PI_LO = 3.1415925

PARAM_SHAPES = dict(
    w_mem_kv=(1024, 2048), l0_w_in=(1024, 2560), l0_sgu_ln_g=(512,), l0_sgu_ln_b=(512,),
    l0_w_spatial=(4, 128, 128), l0_b_spatial=(4, 128), l0_lam_q1=(64,), l0_lam_k1=(64,),
    l0_lam_q2=(64,), l0_lam_k2=(64,), l0_subln_g=(128,), l0_w_out=(1024, 1024),
    l1_w_in=(1024, 1024), l1_log_dt=(64,), l1_lambda_re=(64, 64), l1_lambda_im=(64, 64),
    l1_b_re=(64, 64, 16), l1_b_im=(64, 64, 16), l1_c_re=(64, 16, 64), l1_c_im=(64, 16, 64),
    l1_d_skip=(64, 16), l1_w_val=(1024, 1024), l1_w_gate=(1024, 1024),
)
for _l in ("l0_", "l1_"):
    PARAM_SHAPES.update({
        _l + "ln1_g": (1024,), _l + "ln1_b": (1024,), _l + "xq": (1024, 1024), _l + "xo": (1024, 1024),
        _l + "ln2_g": (1024,), _l + "ln2_b": (1024,), _l + "router_w": (1024, 32), _l + "router_b": (32,),
        _l + "exp_w_up": (32, 1024, 2048), _l + "exp_b_up": (32, 2048), _l + "exp_w_down": (32, 1024, 1024),
        _l + "exp_b_down": (32, 1024), _l + "ln3_g": (1024,), _l + "ln3_b": (1024,),
    })


class Tile:
    __slots__ = ("ap", "w", "r", "name")

    def __init__(self, ap, name=""):
        self.ap = ap
        self.w = None
        self.r = []
        self.name = name

    def __getitem__(self, k):
        return self.ap[k]


class Eng:
    def __init__(self, K, name, obj):
        self.name = name
        self.obj = obj
        self.sem = K.newsem(name)
        self.count = 0
        self.waited = {}
        self.dsems = None
        self.di = 0


class Sched:
    def __init__(self, nc, es):
        self.nc = nc
        self.es = es
        self.nsem = 0
        self.E = {}
        for n, o in (("pe", nc.tensor), ("act", nc.scalar), ("dve", nc.vector),
                     ("pool", nc.gpsimd), ("sp", nc.sync)):
            self.E[n] = Eng(self, n, o)
        self.ninst = 0

    def newsem(self, name):
        self.nsem += 1
        return self.es.enter_context(self.nc.semaphore("s%s%d" % (name, self.nsem)))

    def _deps(self, outs, ins):
        deps = []
        for t in ins:
            if t.w is not None:
                if isinstance(t.w, list):
                    deps.extend(t.w)
                else:
                    deps.append(t.w)
        for t in outs:
            if t.w is not None:
                if isinstance(t.w, list):
                    deps.extend(t.w)
                else:
                    deps.append(t.w)
            deps.extend(t.r)
        return deps

    def _wait(self, eng, deps):
        e = self.E[eng]
        for (pe_, sem, val) in deps:
            if pe_ == "pe" and eng == "pe":
                continue
            key = sem.num
            if e.waited.get(key, 0) >= val:
                continue
            e.obj.wait_ge(sem, val)
            e.waited[key] = val
            self.ninst += 1

    def op(self, eng, fn, outs=(), ins=(), inc=True):
        e = self.E[eng]
        self._wait(eng, self._deps(outs, ins))
        inst = fn(e.obj)
        self.ninst += 1
        if e.count >= SEM_ROLL and inc:
            e.sem = self.newsem(eng)
            e.count = 0
        if inc:
            e.count += 1
            inst.then_inc(e.sem, 1)
            rec = (eng, e.sem, e.count)
        else:
            rec = (eng, e.sem, e.count + 1)
        for t in ins:
            t.r.append(rec)
        for t in outs:
            t.w = rec
            t.r = []
        return inst

    def dma(self, q, out_ap, in_ap, outs=(), ins=(), **kw):
        return self.dmaf(q, lambda e: e.dma_start(out=out_ap, in_=in_ap, **kw), outs=outs, ins=ins)

    def dmaf(self, q, fn, outs=(), ins=()):
        e = self.E[q]
        if e.dsems is None:
            e.dsems = [[self.newsem(q + "d"), 0] for _ in range(NDS)]
        self._wait(q, self._deps(outs, ins))
        slot = e.dsems[e.di % NDS]
        e.di += 1
        if slot[1] >= SEM_ROLL:
            slot[0] = self.newsem(q + "d")
            slot[1] = 0
        sem, tot = slot
        if tot > 0 and e.waited.get(sem.num, 0) < tot:
            e.obj.wait_ge(sem, tot)
            e.waited[sem.num] = tot
        inst = fn(e.obj)
        inst.then_inc(sem, 16)
        slot[1] = tot + 16
        rec = ("dma", sem, tot + 16)
        self.last_rec = rec
        self.ninst += 1
        for t in ins:
            t.r.append(rec)
        for t in outs:
            t.w = rec
            t.r = []
        return inst

    def pre(self, eng, ins):
        self._wait(eng, self._deps((), ins))

    def barrier(self):
        recs = []
        for n, e in self.E.items():
            if e.count > 0:
                recs.append((n + "_b", e.sem, e.count))
            if e.dsems:
                for sem, tot in e.dsems:
                    if tot > 0:
                        recs.append(("dma", sem, tot))
        for n in self.E:
            self._wait(n, recs)


class Ctx:
    pass


def sb(C, st, shape, dt=F32, name="t"):
    C.n += 1
    t = st.enter_context(C.nc.sbuf_tensor("%s_%d" % (name, C.n), list(shape), dt))
    return Tile(t, name)


def mm(C, out_ap, lhsT, rhs, start, stop, outs, ins, inc=None):
    if inc is None:
        inc = stop
    C.K.op("pe", lambda e: e.matmul(out_ap, lhsT, rhs, start=start, stop=stop), outs=outs, ins=ins, inc=inc)


def tt(C, eng, out_ap, a, b, op, outs, ins):
    C.K.op(eng, lambda e: e.tensor_tensor(out=out_ap, in0=a, in1=b, op=op), outs=outs, ins=ins)


def ts(C, eng, out_ap, a, s1, s2, op0, op1, outs, ins):
    if op1 is None:
        C.K.op(eng, lambda e: e.tensor_scalar(out=out_ap, in0=a, scalar1=s1, scalar2=None, op0=op0), outs=outs, ins=ins)
    else:
        C.K.op(eng, lambda e: e.tensor_scalar(out=out_ap, in0=a, scalar1=s1, scalar2=s2, op0=op0, op1=op1), outs=outs, ins=ins)


def stt(C, out_ap, a, scalar, b, op0, op1, outs, ins):
    C.K.op("dve", lambda e: e.scalar_tensor_tensor(out=out_ap, in0=a, scalar=scalar, in1=b, op0=op0, op1=op1),
           outs=outs, ins=ins)


def act(C, out_ap, in_ap, func, outs, ins, scale=1.0, bias=None):
    if bias is None:
        C.K.op("act", lambda e: e.activation(out=out_ap, in_=in_ap, func=func, scale=scale), outs=outs, ins=ins)
    else:
        C.K.op("act", lambda e: e.activation(out=out_ap, in_=in_ap, func=func, scale=scale, bias=bias), outs=outs, ins=ins)


def cp(C, eng, out_ap, in_ap, outs, ins):
    if eng == "act":
        act(C, out_ap, in_ap, AF.Copy, outs, ins)
    else:
        C.K.op(eng, lambda e: e.tensor_copy(out_ap, in_ap), outs=outs, ins=ins)


def load_wbf(C, dst, src2d, ncols, col0=0, kts=8):
    c = 0
    recs = []
    first = True
    while c < ncols:
        n = min(512, ncols - c)
        C.K.dma("pool", dst[:, :, c:c + n],
                src2d[:, col0 + c:col0 + c + n].rearrange("(kt p) n -> p kt n", p=128), outs=([dst] if first else []))
        recs.append(C.K.last_rec)
        first = False
        c += n
    dst.w = recs
    dst.r = []


def load_bc(C, dst, vec):
    C.K.dma("sp", dst[:], vec.partition_broadcast(128), outs=[dst])


def transpose8(C, src, dst, dst_ap_fn, psA, psB, eng0="dve", eng1="act", extra=None):
    for kt in range(8):
        bank = psA if kt < 4 else psB
        C.K.op("pe", lambda e: e.transpose(out=bank[:, (kt % 4) * 128:(kt % 4 + 1) * 128],
                                           in_=src[:, kt * 128:(kt + 1) * 128], identity=C.ident[:]),
               outs=[bank], ins=[src, C.ident], inc=(kt % 4 == 3))
    cp(C, eng0, dst_ap_fn(0), psA[:].rearrange("p (k n) -> p k n", k=4), [dst], [psA])
    if extra is not None:
        extra(0, psA)
    cp(C, eng1, dst_ap_fn(4), psB[:].rearrange("p (k n) -> p k n", k=4), [dst], [psB])
    if extra is not None:
        extra(4, psB)


class LNTmp:
    def __init__(self, C, st):
        self.st6 = sb(C, st, [128, 2, 6], F32, "lnst")
        self.mv = sb(C, st, [128, 2], F32, "lnmv")
        self.vp = sb(C, st, [128, 1], F32, "lnvp")
        self.rs = sb(C, st, [128, 1], F32, "lnrs")
        self.zn = sb(C, st, [128, 1024], F32, "lnzn")


def layernorm(C, z, out, T, g_bc, b_bc, rs_eng="act"):
    K = C.K
    for c in range(2):
        K.op("dve", lambda e: e.bn_stats(out=T.st6[:, c, :], in_=z[:, c * 512:(c + 1) * 512]), outs=[T.st6], ins=[z])
    K.op("dve", lambda e: e.bn_aggr(out=T.mv[:], in_=T.st6[:].rearrange("p a b -> p (a b)")), outs=[T.mv], ins=[T.st6])
    ts(C, "dve", T.vp[:], T.mv[:, 1:2], EPS, None, ALU.add, None, [T.vp], [T.mv])
    if rs_eng == "act":
        act(C, T.rs[:], T.vp[:], AF.Ln, [T.rs], [T.vp])
        act(C, T.rs[:], T.rs[:], AF.Exp, [T.rs], [T.rs], scale=-0.5)
    else:
        tt(C, "pool", T.rs[:], T.vp[:], C.neghalf[:, 0:1], ALU.pow, [T.rs], [T.vp, C.neghalf])
    stt(C, T.zn[:], z[:], T.mv[:, 0:1], g_bc[:], ALU.subtract, ALU.mult, [T.zn], [z, T.mv, g_bc])
    stt(C, out[:], T.zn[:], T.rs[:, 0:1], b_bc[:], ALU.mult, ALU.add, [out], [T.zn, T.rs, b_bc])


class GeluTmp:
    def __init__(self, C, st, n):
        self.xs = sb(C, st, [128, n], F32, "gxs")


def gelu(C, src_ap, src_tiles, out_ap, out_tiles, T, n, xs_given=False):
    if xs_given:
        act(C, out_ap, T.xs[:, 0:n], AF.Gelu_apprx_tanh, out_tiles, [T.xs])
    else:
        act(C, out_ap, src_ap, AF.Gelu_apprx_tanh, out_tiles, src_tiles)


def phase_consts(C):
    K = C.K
    st = C.es
    C.ident = sb(C, st, [128, 128], F32, "ident")
    C.ones_f = sb(C, st, [128, 128], F32, "onesf")
    C.ones_b = sb(C, st, [128, 128], BF16, "onesb")
    C.neghalf = sb(C, st, [128, 512], F32, "neghalf")
    C.nident = sb(C, st, [128, 128], F32, "nident")
    C.kmT = sb(C, st, [128, 2, 8, 256], BF16, "kmT")
    C.vmem = sb(C, st, [128, 2, 2, 1024], BF16, "vmem")
    K.op("pool", lambda e: e.memset(C.ones_f[:], 1.0), outs=[C.ones_f])
    K.op("pool", lambda e: e.memset(C.ones_b[:], 1.0), outs=[C.ones_b])
    K.op("pool", lambda e: e.memset(C.neghalf[:], -0.5), outs=[C.neghalf])
    K.op("pool", lambda e: e.affine_select(out=C.ident[:], in_=C.ones_f[:], pattern=[[-1, 128]],
                                           compare_op=ALU.is_equal, fill=0.0, base=0, channel_multiplier=1),
         outs=[C.ident], ins=[C.ones_f])
    K.op("pool", lambda e: e.tensor_scalar(out=C.nident[:], in0=C.ident[:], scalar1=-1.0, scalar2=None, op0=ALU.mult),
         outs=[C.nident], ins=[C.ident])


def phase_kv(C):
    K = C.K
    P = C.P
    with ExitStack() as st:
        wkv = sb(C, st, [128, 8, 2048], BF16, "wkv")
        load_wbf(C, wkv, P["w_mem_kv"], 2048)
        mt_ = [sb(C, st, [128, 1024], F32, "memt") for _ in range(2)]
        memT = sb(C, st, [128, 8, 256], BF16, "memT")
        for s in range(2):
            for mt in range(2):
                t = mt_[mt]
                K.dma("sp", t[:], P["mem"][s * 256 + mt * 128:s * 256 + (mt + 1) * 128, :], outs=[t])
                transpose8(C, t, memT, lambda k0: memT[:, k0:k0 + 4, mt * 128:(mt + 1) * 128], C.PS[0], C.PS[1])
            for ft in range(8):
                ps = C.PS[2 + ft % 2]
                for kt in range(8):
                    mm(C, ps[:, 0:256], wkv[:, kt, ft * 128:(ft + 1) * 128], memT[:, kt, :], kt == 0, kt == 7,
                       [ps], [wkv, memT])
                act(C, C.kmT[:, s, ft, :], ps[:, 0:256], AF.Copy, [C.kmT], [ps], scale=1.0 / 16.0)
            for mt in range(2):
                for half in range(2):
                    ps = C.PS[4 + half]
                    for kt in range(8):
                        mm(C, ps[:], memT[:, kt, mt * 128:(mt + 1) * 128],
                           wkv[:, kt, 1024 + half * 512:1024 + (half + 1) * 512], kt == 0, kt == 7, [ps], [memT, wkv])
                    cp(C, "dve", C.vmem[:, s, mt, half * 512:(half + 1) * 512], ps[:], [C.vmem], [ps])
        K.barrier()


def phase_l0_attn(C, s, dT, src):
    K = C.K
    P = C.P
    with ExitStack() as st:
        wqkv = sb(C, st, [128, 8, 1536], BF16, "wqkv")
        load_wbf(C, wqkv, P["l0_w_in"], 1536, col0=1024)
        qTa = sb(C, st, [128, 4, 2048], BF16, "qT")
        kTa = sb(C, st, [128, 4, 2048], BF16, "kT")
        vda = sb(C, st, [128, 16, 512], BF16, "vd")
        qT = [Tile(qTa.ap) for _ in range(4)]
        kT = [Tile(kTa.ap) for _ in range(4)]
        vd = [Tile(vda.ap) for _ in range(4)]
        xt = [sb(C, st, [128, 1024], F32, "xt") for _ in range(3)]
        xTb = [sb(C, st, [128, 8, 512], BF16, "xTb") for _ in range(2)]
        lv = [sb(C, st, [128, 64], F32, "lv") for _ in range(4)]
        for i, nme in enumerate(("l0_lam_q1", "l0_lam_k1", "l0_lam_q2", "l0_lam_k2")):
            load_bc(C, lv[i], P[nme])
        l1 = sb(C, st, [128, 2], F32, "l1")
        neglam = sb(C, st, [128, 1], F32, "neglam")
        gsub = sb(C, st, [128, 1], F32, "gsub")
        tt(C, "dve", lv[0][:], lv[0][:], lv[1][:], ALU.mult, [lv[0]], [lv[0], lv[1]])
        tt(C, "dve", lv[2][:], lv[2][:], lv[3][:], ALU.mult, [lv[2]], [lv[2], lv[3]])
        K.op("dve", lambda e: e.reduce_sum(out=l1[:, 0:1], in_=lv[0][:], axis=AX.X), outs=[l1], ins=[lv[0]])
        K.op("dve", lambda e: e.reduce_sum(out=l1[:, 1:2], in_=lv[2][:], axis=AX.X), outs=[l1], ins=[lv[2]])
        act(C, l1[:], l1[:], AF.Exp, [l1], [l1])
        tt(C, "dve", neglam[:], l1[:, 1:2], l1[:, 0:1], ALU.subtract, [neglam], [l1])
        ts(C, "dve", neglam[:], neglam[:], -LAM_INIT, None, ALU.add, None, [neglam], [neglam])
        K.dma("sp", gsub[:], P["l0_subln_g"].rearrange("(p o) -> p o", o=1), outs=[gsub])
        ts(C, "dve", gsub[:], gsub[:], 1.0 - LAM_INIT, None, ALU.mult, None, [gsub], [gsub])

        nx = 0
        for blk in range(4):
            xb_ = xTb[blk % 2]
            for t4 in range(4):
                x = xt[nx % 3]
                nx += 1
                r0 = s * SEQ + blk * 512 + t4 * 128
                K.dma("sp", x[:], src[r0:r0 + 128, :], outs=[x])
                transpose8(C, x, xb_, lambda k0: xb_[:, k0:k0 + 4, t4 * 128:(t4 + 1) * 128], C.PS[0], C.PS[1])
            for h in range(4):
                ps = C.PS[2 + h % 2]
                for kt in range(8):
                    mm(C, ps[:], wqkv[:, kt, h * 128:(h + 1) * 128], xb_[:, kt, :], kt == 0, kt == 7, [ps], [wqkv, xb_])
                act(C, qTa[:, h, blk * 512:(blk + 1) * 512], ps[:], AF.Copy, [qT[blk]], [ps], scale=0.125)
                ps = C.PS[4 + h % 2]
                for kt in range(8):
                    mm(C, ps[:], wqkv[:, kt, 512 + h * 128:512 + (h + 1) * 128], xb_[:, kt, :], kt == 0, kt == 7,
                       [ps], [wqkv, xb_])
                cp(C, "dve", kTa[:, h, blk * 512:(blk + 1) * 512], ps[:], [kT[blk]], [ps])
            for t4 in range(4):
                ps = C.PS[6 + t4 % 2]
                for kt in range(8):
                    mm(C, ps[:], xb_[:, kt, t4 * 128:(t4 + 1) * 128], wqkv[:, kt, 1024:1536], kt == 0, kt == 7,
                       [ps], [xb_, wqkv])
                cp(C, "act" if t4 % 2 else "dve", vda[:, blk * 4 + t4, :], ps[:], [vd[blk]], [ps])

        pT = [sb(C, st, [128, 512], BF16, "pT") for _ in range(4)]
        rc = [sb(C, st, [128, 512], F32, "rc") for _ in range(2)]
        tq = [sb(C, st, [128, 512], F32, "tq") for _ in range(2)]
        o_ = sb(C, st, [128, 512], F32, "o")
        osq = sb(C, st, [128, 512], BF16, "osq")
        v1 = sb(C, st, [128, 512], F32, "v1")
        rstd = sb(C, st, [128, 512], F32, "rstd")
        NT = [C.PS[2], C.PS[3]]
        DT = [C.PS[4], C.PS[5]]
        cnt = 0
        SB = [C.PS[0], C.PS[1], C.PS[7]]
        for h in range(4):
            for qb in range(4):
                last = 4 * qb + 3
                items = [(kt, c) for kt in range(last + 1) for c in range(2)]

                def score(kt, c, n):
                    i = kt - 4 * qb
                    q0 = max(i, 0) * 128
                    kb = kt // 4
                    sbk = SB[n % 3]
                    p = pT[n % 4]
                    mm(C, sbk[:, q0:512], kTa[c * 64:(c + 1) * 64, h, kt * 128:(kt + 1) * 128],
                       qTa[c * 64:(c + 1) * 64, h, qb * 512 + q0:(qb + 1) * 512], True, True,
                       [sbk], [kT[kb], qT[qb]])
                    act(C, p[:, q0:512], sbk[:, q0:512], AF.Exp, [p], [sbk])
                    if i >= 0:
                        K.op("pool", lambda e: e.affine_select(out=p[:, q0:q0 + 128], in_=p[:, q0:q0 + 128],
                                                               pattern=[[1, 128]], compare_op=ALU.is_ge, fill=0.0,
                                                               base=0, channel_multiplier=-1), outs=[p], ins=[p])

                def accum(kt, c, n):
                    i = kt - 4 * qb
                    q0 = max(i, 0) * 128
                    kb = kt // 4
                    p = pT[n % 4]
                    mm(C, NT[c][:, q0:512], vda[:, kt, h * 128:(h + 1) * 128], p[:, q0:512], kt == 0, kt == last,
                       [NT[c]], [vd[kb], p], inc=(kt == last))
                    mm(C, DT[c][:, q0:512], C.ones_b[:], p[:, q0:512], kt == 0, kt == last,
                       [DT[c]], [C.ones_b, p], inc=True)

                score(items[0][0], items[0][1], cnt)
                for ii, (kt, c) in enumerate(items):
                    if ii + 1 < len(items):
                        score(items[ii + 1][0], items[ii + 1][1], cnt + 1)
                    accum(kt, c, cnt)
                    cnt += 1
                for c in range(2):
                    act(C, rc[c][:], DT[c][:], AF.Ln, [rc[c]], [DT[c]])
                    act(C, rc[c][:], rc[c][:], AF.Exp, [rc[c]], [rc[c]], scale=-1.0)
                    tt(C, "dve", tq[c][:], NT[c][:], rc[c][:], ALU.mult, [tq[c]], [NT[c], rc[c]])
                stt(C, o_[:], tq[1][:], neglam[:, 0:1], tq[0][:], ALU.mult, ALU.add, [o_], [tq[0], tq[1], neglam])
                tt(C, "dve", osq[:], o_[:], o_[:], ALU.mult, [osq], [o_])
                mm(C, C.PS[6][:], C.ones_b[:], osq[:], True, True, [C.PS[6]], [C.ones_b, osq])
                ts(C, "dve", v1[:], C.PS[6][:], 1.0 / 128.0, EPS, ALU.mult, ALU.add, [v1], [C.PS[6]])
                act(C, v1[:], v1[:], AF.Ln, [v1], [v1])
                act(C, rstd[:], v1[:], AF.Exp, [rstd], [v1], scale=-0.5)
                stt(C, dT[:, h, qb * 512:(qb + 1) * 512], o_[:], gsub[:, 0:1], rstd[:], ALU.mult, ALU.mult,
                    [dT], [o_, gsub, rstd])
        K.barrier()


def phase_l0_sgu_out(C, s, dT, src, dst):
    K = C.K
    P = C.P
    with ExitStack() as st:
        wuv = sb(C, st, [128, 8, 1024], BF16, "wuv")
        load_wbf(C, wuv, P["l0_w_in"], 1024, col0=0)
        wout = sb(C, st, [128, 8, 1024], BF16, "wout")
        load_wbf(C, wout, P["l0_w_out"], 1024)
        g1 = sb(C, st, [128, 1024], F32, "g1")
        b1 = sb(C, st, [128, 1024], F32, "b1")
        load_bc(C, g1, P["l0_ln1_g"])
        load_bc(C, b1, P["l0_ln1_b"])
        sg_ = sb(C, st, [128, 512], F32, "sg")
        sbb = sb(C, st, [128, 512], F32, "sbb")
        bsp = sb(C, st, [128, 512], F32, "bsp")
        load_bc(C, sg_, P["l0_sgu_ln_g"])
        load_bc(C, sbb, P["l0_sgu_ln_b"])
        load_bc(C, bsp, P["l0_b_spatial"].rearrange("g t -> (g t)"))
        wsp = sb(C, st, [128, 4, 128], F32, "wsp")
        WTf = sb(C, st, [128, 4, 128], F32, "WTf")
        WT = sb(C, st, [128, 4, 128], BF16, "WT")
        K.dma("sp", wsp[:], P["l0_w_spatial"].rearrange("g t s -> t g s"), outs=[wsp])
        for g in range(4):
            K.op("pe", lambda e: e.transpose(out=C.PS[0][:, g * 128:(g + 1) * 128], in_=wsp[:, g, :], identity=C.ident[:]),
                 outs=[C.PS[0]], ins=[wsp, C.ident], inc=(g == 3))
        cp(C, "dve", WTf[:], C.PS[0][:].rearrange("p (g t) -> p g t", g=4), [WTf], [C.PS[0]])
        K.op("pool", lambda e: e.affine_select(out=WT[:], in_=WTf[:], pattern=[[0, 4], [1, 128]], compare_op=ALU.is_ge,
                                               fill=0.0, base=0, channel_multiplier=-1), outs=[WT], ins=[WTf])
        xt = [sb(C, st, [128, 1024], F32, "xt") for _ in range(8)]
        xTb = [sb(C, st, [128, 8, 512], BF16, "xTb") for _ in range(2)]
        ug = [sb(C, st, [128, 4, 512], BF16, "ug") for _ in range(2)]
        GTs = [GeluTmp(C, st, 512) for _ in range(2)]
        vgs = [sb(C, st, [128, 512], F32, "vg") for _ in range(2)]
        st4s = [sb(C, st, [128, 4, 6], F32, "st4") for _ in range(2)]
        mv4s = [sb(C, st, [128, 4, 2], F32, "mv4") for _ in range(2)]
        vp4s = [sb(C, st, [128, 4], F32, "vp4") for _ in range(2)]
        rs4s = [sb(C, st, [128, 4], F32, "rs4") for _ in range(2)]
        vns = [sb(C, st, [128, 512], BF16, "vn") for _ in range(2)]
        gss = [sb(C, st, [128, 512], F32, "gs") for _ in range(2)]
        aT = [sb(C, st, [128, 4, 128], BF16, "aT") for _ in range(2)]
        zs = [sb(C, st, [128, 1024], F32, "z") for _ in range(2)]
        LTs = [LNTmp(C, st) for _ in range(2)]
        ot = [sb(C, st, [128, 1024], F32, "ot") for _ in range(2)]
        no = 0
        ngl = 0

        def load_block(blk):
            xb2 = xTb[blk % 2]
            for t4 in range(4):
                x = xt[(blk % 2) * 4 + t4]
                r0 = s * SEQ + blk * 512 + t4 * 128
                K.dma("sp", x[:], src[r0:r0 + 128, :], outs=[x])
                transpose8(C, x, xb2, lambda k0: xb2[:, k0:k0 + 4, t4 * 128:(t4 + 1) * 128], C.PS[0], C.PS[1])

        load_block(0)
        for blk in range(4):
            xb_ = xTb[blk % 2]
            xs_ = [xt[(blk % 2) * 4 + t4] for t4 in range(4)]
            u = ug[blk % 2]
            for g in range(4):
                ps = C.PS[2 + g % 2]
                for kt in range(8):
                    mm(C, ps[:], wuv[:, kt, g * 128:(g + 1) * 128], xb_[:, kt, :], kt == 0, kt == 7, [ps], [wuv, xb_])
                gelu(C, ps[:], [ps], u[:, g, :], [u], GTs[ngl % 2], 512)
                ngl += 1
            if blk + 1 < 4:
                load_block(blk + 1)
            def stage1(t4):
                GT = GTs[t4 % 2]
                vg, st4, mv4, vp4, rs4 = vgs[t4 % 2], st4s[t4 % 2], mv4s[t4 % 2], vp4s[t4 % 2], rs4s[t4 % 2]
                vn = vns[t4 % 2]
                ps = C.PS[4]
                for kt in range(8):
                    mm(C, ps[:], xb_[:, kt, t4 * 128:(t4 + 1) * 128], wuv[:, kt, 512:1024], kt == 0, kt == 7,
                       [ps], [xb_, wuv])
                gelu(C, ps[:], [ps], vg[:], [vg], GT, 512)
                for g in range(4):
                    K.op("dve", lambda e: e.bn_stats(out=st4[:, g, :], in_=vg[:, g * 128:(g + 1) * 128]),
                         outs=[st4], ins=[vg])
                for g in range(4):
                    K.op("dve", lambda e: e.bn_aggr(out=mv4[:, g, :], in_=st4[:, g, :]), outs=[mv4], ins=[st4])
                ts(C, "dve", vp4[:], mv4[:, :, 1], EPS, None, ALU.add, None, [vp4], [mv4])
                act(C, rs4[:], vp4[:], AF.Ln, [rs4], [vp4])
                act(C, rs4[:], rs4[:], AF.Exp, [rs4], [rs4], scale=-0.5)
                for g in range(4):
                    gsl = slice(g * 128, (g + 1) * 128)
                    stt(C, vg[:, gsl], vg[:, gsl], mv4[:, g, 0:1], sg_[:, gsl], ALU.subtract, ALU.mult, [vg], [vg, mv4, sg_])
                    stt(C, vn[:, gsl], vg[:, gsl], rs4[:, g:g + 1], sbb[:, gsl], ALU.mult, ALU.add, [vn], [vg, rs4, sbb])

            def stage2(t4):
                nonlocal no
                x = xs_[t4]
                vn, gs, z, LT = vns[t4 % 2], gss[t4 % 2], zs[t4 % 2], LTs[t4 % 2]
                psg = C.PS[5]
                for g in range(4):
                    mm(C, psg[:, g * 128:(g + 1) * 128], vn[:, g * 128:(g + 1) * 128], WT[:, g, :], True, True,
                       [psg], [vn, WT], inc=(g == 3))
                tt(C, "dve", gs[:], psg[:], bsp[:], ALU.add, [gs], [psg, bsp])
                a = aT[t4 % 2]
                tt(C, "pool", a[:], gs[:].rearrange("p (g t) -> p g t", g=4), u[:, :, t4 * 128:(t4 + 1) * 128], ALU.mult,
                   [a], [gs, u])
                tok0 = blk * 512 + t4 * 128
                for half in range(2):
                    ps = C.PS[6 + half]
                    for ft in range(4):
                        mm(C, ps[:], a[:, ft, :], wout[:, ft, half * 512:(half + 1) * 512], ft == 0, False,
                           [ps], [a, wout], inc=False)
                    for ft in range(4):
                        mm(C, ps[:], dT[:, ft, tok0:tok0 + 128], wout[:, 4 + ft, half * 512:(half + 1) * 512], False,
                           ft == 3, [ps], [dT, wout], inc=(ft == 3))
                    stt(C, z[:, half * 512:(half + 1) * 512], x[:, half * 512:(half + 1) * 512], ALPHA, ps[:],
                        ALU.mult, ALU.add, [z], [x, ps])
                o = ot[no % 2]
                no += 1
                layernorm(C, z, o, LT, g1, b1)
                r0 = s * SEQ + tok0
                K.dma("sp", dst[r0:r0 + 128, :], o[:], ins=[o])

            stage1(0)
            for t4 in range(4):
                if t4 + 1 < 4:
                    stage1(t4 + 1)
                stage2(t4)
        K.barrier()


def phase_cross(C, L, src, dst):
    K = C.K
    P = C.P
    with ExitStack() as st:
        wxq = sb(C, st, [128, 8, 1024], BF16, "wxq")
        wxo = sb(C, st, [128, 8, 1024], BF16, "wxo")
        load_wbf(C, wxq, P[L + "xq"], 1024)
        load_wbf(C, wxo, P[L + "xo"], 1024)
        g2 = sb(C, st, [128, 1024], F32, "g2")
        b2 = sb(C, st, [128, 1024], F32, "b2")
        load_bc(C, g2, P[L + "ln2_g"])
        load_bc(C, b2, P[L + "ln2_b"])
        xt = [sb(C, st, [128, 1024], F32, "xt") for _ in range(8)]
        xTb = [sb(C, st, [128, 8, 512], BF16, "xTb") for _ in range(2)]
        qT = [sb(C, st, [128, 8, 512], BF16, "qT") for _ in range(2)]
        pT = [sb(C, st, [128, 2, 512], BF16, "pT") for _ in range(2)]
        oT = [sb(C, st, [128, 8, 512], BF16, "oT") for _ in range(2)]
        rcs = [sb(C, st, [128, 512], F32, "rc") for _ in range(2)]
        zs = [sb(C, st, [128, 1024], F32, "z") for _ in range(2)]
        LTs = [LNTmp(C, st) for _ in range(2)]
        ot = [sb(C, st, [128, 1024], F32, "ot") for _ in range(2)]
        no = [0]

        def stageA(blk, after_head=None):
            s = blk // 4
            xb_ = xTb[blk % 2]
            for t4 in range(4):
                x = xt[(blk % 2) * 4 + t4]
                r0 = blk * 512 + t4 * 128
                K.dma("sp", x[:], src[r0:r0 + 128, :], outs=[x])
                transpose8(C, x, xb_, lambda k0: xb_[:, k0:k0 + 4, t4 * 128:(t4 + 1) * 128], C.PS[0], C.PS[1])
            q = qT[blk % 2]
            for ft in range(8):
                ps = C.PS[2 + ft % 2]
                for kt in range(8):
                    mm(C, ps[:], wxq[:, kt, ft * 128:(ft + 1) * 128], xb_[:, kt, :], kt == 0, kt == 7, [ps], [wxq, xb_])
                cp(C, "act" if ft % 2 else "dve", q[:, ft, :], ps[:], [q], [ps])
            o_ = oT[blk % 2]

            def sc(h):
                p = pT[h % 2]
                for mt in range(2):
                    ps = C.PS[4 + mt]
                    for j in range(2):
                        mm(C, ps[:], C.kmT[:, s, h * 2 + j, mt * 128:(mt + 1) * 128], q[:, h * 2 + j, :], j == 0, j == 1,
                           [ps], [C.kmT, q])
                    act(C, p[:, mt, :], ps[:], AF.Exp, [p], [ps])

            def pv(h):
                p = pT[h % 2]
                rc = rcs[h % 2]
                psd = C.PS[6]
                for mt in range(2):
                    mm(C, psd[:], C.ones_b[:], p[:, mt, :], mt == 0, mt == 1, [psd], [C.ones_b, p])
                act(C, rc[:], psd[:], AF.Ln, [rc], [psd])
                act(C, rc[:], rc[:], AF.Exp, [rc], [rc], scale=-1.0)
                for j in range(2):
                    ps = C.PS[(7, 0)[j]]
                    for mt in range(2):
                        mm(C, ps[:], C.vmem[:, s, mt, h * 256 + j * 128:h * 256 + (j + 1) * 128], p[:, mt, :], mt == 0,
                           mt == 1, [ps], [C.vmem, p])
                    tt(C, "dve", o_[:, h * 2 + j, :], ps[:], rc[:], ALU.mult, [o_], [ps, rc])

            sc(0)
            for h in range(4):
                if h + 1 < 4:
                    sc(h + 1)
                pv(h)
                if after_head is not None:
                    after_head(h)

        def stageB(blk, only=None):
            o_ = oT[blk % 2]
            for t4 in (range(4) if only is None else [only]):
                x = xt[(blk % 2) * 4 + t4]
                z = zs[t4 % 2]
                LT = LTs[t4 % 2]
                for half in range(2):
                    ps = C.PS[2 + half]
                    for ft in range(8):
                        mm(C, ps[:], o_[:, ft, t4 * 128:(t4 + 1) * 128], wxo[:, ft, half * 512:(half + 1) * 512], ft == 0,
                           ft == 7, [ps], [o_, wxo])
                    stt(C, z[:, half * 512:(half + 1) * 512], x[:, half * 512:(half + 1) * 512], ALPHA, ps[:],
                        ALU.mult, ALU.add, [z], [x, ps])
                o = ot[no[0] % 2]
                no[0] += 1
                layernorm(C, z, o, LT, g2, b2, rs_eng="act")
                r0 = blk * 512 + t4 * 128
                K.dma("sp", dst[r0:r0 + 128, :], o[:], ins=[o])

        stageA(0)
        for blk in range(8):
            if blk + 1 < 8:
                stageA(blk + 1, after_head=lambda h, b=blk: stageB(b, only=h))
            else:
                stageB(blk)
        K.barrier()


NRING = 10


def phase_moe(C, L, src, dst):
    K = C.K
    P = C.P
    with ExitStack() as st:
        g3 = sb(C, st, [128, 1024], F32, "g3")
        b3 = sb(C, st, [128, 1024], F32, "b3")
        load_bc(C, g3, P[L + "ln3_g"])
        load_bc(C, b3, P[L + "ln3_b"])
        rw = sb(C, st, [128, 8, 32], F32, "rw")
        K.dma("sp", rw[:], P[L + "router_w"].rearrange("(kt p) e -> p kt e", p=128), outs=[rw])
        rb = sb(C, st, [128, 32], F32, "rb")
        load_bc(C, rb, P[L + "router_b"])
        bd = sb(C, st, [32, 1024], BF16, "bd")
        bupT = sb(C, st, [128, 16, 32], F32, "bupT")
        with ExitStack() as st2:
            bdf = sb(C, st2, [32, 1024], F32, "bdf")
            K.dma("sp", bdf[:], P[L + "exp_b_down"], outs=[bdf])
            cp(C, "dve", bd[:], bdf[:], [bd], [bdf])
            buf_ = sb(C, st2, [32, 2048], F32, "buf")
            K.dma("sp", buf_[:], P[L + "exp_b_up"], outs=[buf_])
            for ft in range(16):
                ps = C.PS[ft % 2]
                K.op("pe", lambda e: e.transpose(out=ps[:, 0:32], in_=buf_[:, ft * 128:(ft + 1) * 128],
                                                 identity=C.ident[0:32, 0:32]), outs=[ps], ins=[buf_, C.ident])
                cp(C, "dve", bupT[:, ft, :], ps[:, 0:32], [bupT], [ps])
            K.barrier()

        ringU = [sb(C, st, [128, 4096], BF16, "ringU") for _ in range(8)]
        ringD = [sb(C, st, [128, 4096], BF16, "ringD") for _ in range(2)]
        xt = [sb(C, st, [128, 1024], F32, "xt") for _ in range(2)]
        x2T = [sb(C, st, [128, 8, 512], BF16, "x2T") for _ in range(1)]
        x2Tf = sb(C, st, [128, 8, 128], F32, "x2Tf")
        yacc = [sb(C, st, [128, 1024], F32, "yacc") for _ in range(4)]
        gates = sb(C, st, [128, 4, 32], F32, "gates")
        gT = sb(C, st, [32, 4, 128], BF16, "gT")
        lg = sb(C, st, [128, 32], F32, "lg")
        mx8 = sb(C, st, [128, 8], F32, "mx8")
        msk = sb(C, st, [128, 32], F32, "msk")
        nmx = sb(C, st, [128, 1], F32, "nmx")
        ssum = sb(C, st, [128, 1], F32, "ssum")
        actT = sb(C, st, [128, 8, 512], BF16, "actT")
        actTt = [Tile(actT.ap) for _ in range(8)]
        ev = [dict(g=sb(C, st, [128, 512], F32, "evg"), s=sb(C, st, [128, 512], F32, "evs"),
                   l=sb(C, st, [128, 512], F32, "evl"), t=sb(C, st, [128, 512], F32, "evt")) for _ in range(2)]
        LT = LNTmp(C, st)
        ot = [sb(C, st, [128, 1024], F32, "ot") for _ in range(2)]
        wup = P[L + "exp_w_up"]
        wdn = P[L + "exp_w_down"]
        nring = [0]

        def load_up(e):
            ups = []
            for c in (0, 2, 1, 3):
                t = ringU[nring[0] % 8]
                nring[0] += 1
                K.dma("pool", t[:].rearrange("p (kt n) -> p kt n", kt=8),
                      wup[e, :, c * 512:(c + 1) * 512].rearrange("(kt p) n -> p kt n", p=128), outs=[t])
                ups.append((c, t))
            ups = dict(ups)
            return [ups[c] for c in range(4)]

        def load_dn(e):
            dns = []
            for c in range(2):
                t = ringD[c]
                K.dma("pool", t[:].rearrange("p (ft n) -> p ft n", ft=4),
                      wdn[e, c * 512:(c + 1) * 512, :].rearrange("(ft p) n -> p ft n", p=128), outs=[t])
                dns.append(t)
            return dns

        no = 0
        nev = 0
        for blk in range(8):
            xT_ = x2T[0]
            for t4 in range(4):
                x = xt[t4 % 2]
                r0 = blk * 512 + t4 * 128
                K.dma("sp", x[:], src[r0:r0 + 128, :], outs=[x])

                def extra(k0, bank):
                    cp(C, "act" if k0 else "dve", x2Tf[:, k0:k0 + 4, :], bank[:].rearrange("p (k n) -> p k n", k=4),
                       [x2Tf], [bank])
                transpose8(C, x, xT_, lambda k0: xT_[:, k0:k0 + 4, t4 * 128:(t4 + 1) * 128], C.PS[6], C.PS[7],
                           extra=extra)
                act(C, yacc[t4][:], x[:], AF.Copy, [yacc[t4]], [x], scale=ALPHA)
                ps = C.PS[6]
                for kt in range(8):
                    mm(C, ps[:, 0:32], x2Tf[:, kt, :], rw[:, kt, :], kt == 0, kt == 7, [ps], [x2Tf, rw])
                tt(C, "dve", lg[:], ps[:, 0:32], rb[:], ALU.add, [lg], [ps, rb])
                K.op("dve", lambda e: e.max(out=mx8[:], in_=lg[:]), outs=[mx8], ins=[lg])
                ts(C, "dve", msk[:], lg[:], mx8[:, 3:4], None, ALU.is_ge, None, [msk], [lg, mx8])
                ts(C, "dve", nmx[:], mx8[:, 0:1], -1.0, None, ALU.mult, None, [nmx], [mx8])
                act(C, lg[:], lg[:], AF.Exp, [lg], [lg, nmx], bias=nmx[:, 0:1])
                tt(C, "dve", lg[:], lg[:], msk[:], ALU.mult, [lg], [lg, msk])
                K.op("dve", lambda e: e.reduce_sum(out=ssum[:], in_=lg[:], axis=AX.X), outs=[ssum], ins=[lg])
                K.op("dve", lambda e: e.reciprocal(ssum[:], ssum[:]), outs=[ssum], ins=[ssum])
                ts(C, "dve", gates[:, t4, :], lg[:], ssum[:, 0:1], None, ALU.mult, None, [gates], [lg, ssum])
                ps = C.PS[7]
                K.op("pe", lambda e: e.transpose(out=ps[0:32, 0:128], in_=gates[:, t4, :], identity=C.ident[:]),
                     outs=[ps], ins=[gates, C.ident])
                cp(C, "dve", gT[:, t4, :], ps[0:32, 0:128], [gT], [ps])
                for half in range(2):
                    ps = C.PS[6 + half]
                    mm(C, ps[:], gT[:, t4, :], bd[:, half * 512:(half + 1) * 512], True, True, [ps], [gT, bd])
                    tt(C, "dve", yacc[t4][:, half * 512:(half + 1) * 512], yacc[t4][:, half * 512:(half + 1) * 512],
                       ps[:], ALU.add, [yacc[t4]], [yacc[t4], ps])
            nxt_u = load_up(0)
            dns = load_dn(0)
            for e_ in range(32):
                ups = nxt_u
                if e_ + 1 < 32:
                    nxt_u = load_up(e_ + 1)
                for j in range(8):
                    E = ev[nev % 2]
                    nev += 1
                    pg = C.PS[(nev % 2) * 2]
                    pl = C.PS[(nev % 2) * 2 + 1]
                    wg = ups[j // 4]
                    wl = ups[2 + j // 4]
                    c0 = (j % 4) * 128
                    for kt in range(8):
                        mm(C, pg[:], wg[:, kt * 512 + c0:kt * 512 + c0 + 128], xT_[:, kt, :], kt == 0, kt == 7,
                           [pg], [wg, xT_])
                    for kt in range(8):
                        mm(C, pl[:], wl[:, kt * 512 + c0:kt * 512 + c0 + 128], xT_[:, kt, :], kt == 0, kt == 7,
                           [pl], [wl, xT_])
                    ts(C, "dve", E["g"][:], pg[:], bupT[:, j, e_:e_ + 1], 7.0, ALU.add, ALU.min, [E["g"]], [pg, bupT])
                    act(C, E["s"][:], E["g"][:], AF.Sigmoid, [E["s"]], [E["g"]], scale=1.702)
                    act(C, E["l"][:], pl[:], AF.Identity, [E["l"]], [pl, bupT], bias=bupT[:, 8 + j, e_:e_ + 1])
                    ts(C, "dve", E["l"][:], E["l"][:], 7.0, -7.0, ALU.min, ALU.max, [E["l"]], [E["l"]])
                    tt(C, "dve", E["t"][:], E["g"][:], E["s"][:], ALU.mult, [E["t"]], [E["g"], E["s"]])
                    stt(C, actT[:, j, :], E["l"][:], 1.0, E["t"][:], ALU.add, ALU.mult, [actTt[j]], [E["l"], E["t"]])
                for t4 in range(4):
                    for half in range(2):
                        ps = C.PS[4 + half]
                        for ft in range(8):
                            wd = dns[ft // 4]
                            f0 = (ft % 4) * 1024 + half * 512
                            mm(C, ps[:], actT[:, ft, t4 * 128:(t4 + 1) * 128], wd[:, f0:f0 + 512], ft == 0, ft == 7,
                               [ps], [actTt[ft], wd])
                        stt(C, yacc[t4][:, half * 512:(half + 1) * 512], ps[:], gates[:, t4, e_:e_ + 1],
                            yacc[t4][:, half * 512:(half + 1) * 512], ALU.mult, ALU.add, [yacc[t4]],
                            [ps, gates, yacc[t4]])
                if e_ + 1 < 32:
                    dns = load_dn(e_ + 1)
            for t4 in range(4):
                o = ot[no % 2]
                no += 1
                layernorm(C, yacc[t4], o, LT, g3, b3)
                r0 = blk * 512 + t4 * 128
                K.dma("sp", dst[r0:r0 + 128, :], o[:], ins=[o])
        K.barrier()


U32 = mybir.dt.uint32
NBLK = 64
BSZ = 512


def phase_moe_sparse(C, L, src, dst, Xs, Ys):
    K = C.K
    P = C.P
    IOA = bass.IndirectOffsetOnAxis
    wup2 = P[L + "exp_w_up"].rearrange("e k n -> (e k) n")
    wdn2 = P[L + "exp_w_down"].rearrange("e k n -> (e k) n")
    with ExitStack() as stp:
        idx_all = sb(C, stp, [128, 32, 4], I32, "idxall")
        gsel_all = sb(C, stp, [128, 32, 4], F32, "gselall")
        idw = sb(C, stp, [128, NBLK, 8], I32, "idw")
        oh = sb(C, stp, [32, NBLK], F32, "oh")
        bupb = sb(C, stp, [32, 2048], BF16, "bupb")
        bdnb = sb(C, stp, [32, 1024], BF16, "bdnb")
        identb = sb(C, stp, [128, 128], BF16, "identb")
        cp(C, "dve", identb[:], C.ident[:], [identb], [C.ident])
        gT_all = sb(C, stp, [32, 32, 128], BF16, "gTall")
        ohb16 = sb(C, stp, [32, NBLK], BF16, "ohb16")
        ringU = [sb(C, stp, [128, 8, 2048], BF16, "ringU") for _ in range(2)]
        ringD = [sb(C, stp, [128, 8, 1024], BF16, "ringD") for _ in range(2)]
        ringUt = [[Tile(r.ap) for _ in range(8)] for r in ringU]
        ringDt = [[Tile(r.ap) for _ in range(8)] for r in ringD]
        bc_reg = C.nc.gpsimd.to_reg(32 * 1024 - 1)

        def load_w(b):
            wu = ringU[b % 2]
            wd = ringD[b % 2]
            for kt in range(8):
                K.dmaf("pool", lambda e: e.indirect_dma_start(out=wu[:, kt, :], out_offset=None, in_=wup2[:, :],
                                                              in_offset=IOA(ap=idw[:, b, kt:kt + 1].bitcast(U32), axis=0),
                                                              bounds_check=bc_reg, oob_is_err=False),
                       outs=[ringUt[b % 2][kt]], ins=[idw])
            for kt in range(8):
                K.dmaf("pool", lambda e: e.indirect_dma_start(out=wd[:, kt, :], out_offset=None, in_=wdn2[:, :],
                                                              in_offset=IOA(ap=idw[:, b, kt:kt + 1].bitcast(U32), axis=0),
                                                              bounds_check=bc_reg, oob_is_err=False),
                       outs=[ringDt[b % 2][kt]], ins=[idw])

        with ExitStack() as st:
            rw = sb(C, st, [128, 8, 32], F32, "rw")
            K.dma("sp", rw[:], P[L + "router_w"].rearrange("(kt p) e -> p kt e", p=128), outs=[rw])
            rb = sb(C, st, [128, 32], F32, "rb")
            load_bc(C, rb, P[L + "router_b"])
            bf_ = sb(C, st, [32, 2048], F32, "bf_")
            K.dma("sp", bf_[:], P[L + "exp_b_up"], outs=[bf_])
            cp(C, "dve", bupb[:], bf_[:], [bupb], [bf_])
            K.dma("sp", bf_[:, 0:1024], P[L + "exp_b_down"], outs=[bf_])
            cp(C, "dve", bdnb[:], bf_[:, 0:1024], [bdnb], [bf_])
            Ust = sb(C, st, [128, 128], BF16, "Ust")
            K.op("pool", lambda e: e.affine_select(out=Ust[:], in_=C.ones_b[:], pattern=[[1, 128]], compare_op=ALU.is_ge,
                                                   fill=0.0, base=-1, channel_multiplier=-1), outs=[Ust], ins=[C.ones_b])
            pid = sb(C, st, [128, 1], F32, "pid")
            K.op("pool", lambda e: e.iota(pid[:], pattern=[[0, 1]], base=0, channel_multiplier=1,
                                          allow_small_or_imprecise_dtypes=True), outs=[pid])
            base = sb(C, st, [128, 32], F32, "base")
            K.op("dve", lambda e: e.memset(base[:], 0.0), outs=[base])
            gates_all = sb(C, st, [128, 32, 32], F32, "gatesall")
            msk_all = sb(C, st, [128, 32, 32], F32, "mskall")
            rank_all = sb(C, st, [128, 32, 32], F32, "rankall")
            xt = [sb(C, st, [128, 1024], F32, "xt") for _ in range(4)]
            xb16 = [sb(C, st, [128, 1024], BF16, "xb16") for _ in range(2)]

            def ldx(t):
                if t < 32:
                    K.dma("sp", xt[t % 4][:], src[t * 128:(t + 1) * 128, :], outs=[xt[t % 4]])
            x2Tfs = [sb(C, st, [128, 8, 128], F32, "x2Tf") for _ in range(2)]
            lgs = [sb(C, st, [128, 32], F32, "lg") for _ in range(2)]
            mx8s = [sb(C, st, [128, 8], F32, "mx8") for _ in range(2)]
            nmxs = [sb(C, st, [128, 1], F32, "nmx") for _ in range(2)]
            ssums = [sb(C, st, [128, 1], F32, "ssum") for _ in range(2)]
            mskbs = [sb(C, st, [128, 32], BF16, "mskb") for _ in range(2)]
            ldx(0)
            ldx(1)
            for t in range(32):
                ldx(t + 2)
                x = xt[t % 4]
                x2Tf, lg, mx8, nmx, ssum, mskb = x2Tfs[t % 2], lgs[t % 2], mx8s[t % 2], nmxs[t % 2], ssums[t % 2], mskbs[t % 2]
                transpose8(C, x, x2Tf, lambda k0: x2Tf[:, k0:k0 + 4, :], C.PS[0], C.PS[1])
                ps = C.PS[2 + t % 2]
                for kt in range(8):
                    mm(C, ps[:, 0:32], x2Tf[:, kt, :], rw[:, kt, :], kt == 0, kt == 7, [ps], [x2Tf, rw])
                tt(C, "dve", lg[:], ps[:, 0:32], rb[:], ALU.add, [lg], [ps, rb])
                K.op("dve", lambda e: e.max(out=mx8[:], in_=lg[:]), outs=[mx8], ins=[lg])
                ts(C, "dve", msk_all[:, t, :], lg[:], mx8[:, 3:4], None, ALU.is_ge, None, [msk_all], [lg, mx8])
                ts(C, "dve", nmx[:], mx8[:, 0:1], -1.0, None, ALU.mult, None, [nmx], [mx8])
                act(C, lg[:], lg[:], AF.Exp, [lg], [lg, nmx], bias=nmx[:, 0:1])
                tt(C, "dve", lg[:], lg[:], msk_all[:, t, :], ALU.mult, [lg], [lg, msk_all])
                K.op("dve", lambda e: e.reduce_sum(out=ssum[:], in_=lg[:], axis=AX.X), outs=[ssum], ins=[lg])
                K.op("dve", lambda e: e.reciprocal(ssum[:], ssum[:]), outs=[ssum], ins=[ssum])
                ts(C, "dve", gates_all[:, t, :], lg[:], ssum[:, 0:1], None, ALU.mult, None, [gates_all], [lg, ssum])
                psg_ = C.PS[6 + t % 2]
                K.op("pe", lambda e: e.transpose(out=psg_[0:32, 0:128], in_=gates_all[:, t, :], identity=C.ident[:]),
                     outs=[psg_], ins=[gates_all, C.ident])
                cp(C, "act", gT_all[:, t, :], psg_[0:32, 0:128], [gT_all], [psg_])
                cp(C, "dve", mskb[:], msk_all[:, t, :], [mskb], [msk_all])
                ps2 = C.PS[4 + t % 2]
                mm(C, ps2[:, 0:32], Ust[:], mskb[:], True, True, [ps2], [Ust, mskb], inc=False)
                mm(C, ps2[:, 32:64], C.ones_b[:], mskb[:], True, True, [ps2], [C.ones_b, mskb], inc=True)
                tt(C, "dve", rank_all[:, t, :], ps2[:, 0:32], base[:], ALU.add, [rank_all], [ps2, base])
                tt(C, "dve", base[:], base[:], ps2[:, 32:64], ALU.add, [base], [base, ps2])
            pad = sb(C, st, [128, 32], F32, "pad")
            padi = sb(C, st, [128, 32], I32, "padi")
            pend = sb(C, st, [128, 32], F32, "pend")
            pstart = sb(C, st, [128, 32], F32, "pstart")
            ts(C, "dve", pad[:], base[:], float(BSZ - 1), 1.0 / BSZ, ALU.add, ALU.mult, [pad], [base])
            ts(C, "dve", pad[:], pad[:], -0.49951171875, None, ALU.add, None, [pad], [pad])
            cp(C, "dve", padi[:], pad[:], [padi], [pad])
            cp(C, "dve", pad[:], padi[:], [pad], [padi])
            ts(C, "dve", pad[:], pad[:], float(BSZ), None, ALU.mult, None, [pad], [pad])
            K.op("dve", lambda e: e.tensor_tensor_scan(out=pend[:], data0=C.ones_f[:, 0:32], data1=pad[:], initial=0.0,
                                                       op0=ALU.mult, op1=ALU.add), outs=[pend], ins=[C.ones_f, pad])
            tt(C, "dve", pstart[:], pend[:], pad[:], ALU.subtract, [pstart], [pend, pad])
            cmp_ = sb(C, st, [128, NBLK, 32], F32, "cmp")
            be = sb(C, st, [128, NBLK], F32, "be")
            for b in range(NBLK):
                ts(C, "dve", cmp_[:, b, :], pend[:], float(BSZ * b), None, ALU.is_le, None, [cmp_], [pend])
            K.op("dve", lambda e: e.reduce_sum(out=be[:], in_=cmp_[:], axis=AX.X), outs=[be], ins=[cmp_])
            idf = sb(C, st, [128, NBLK, 8], F32, "idf")
            for kt in range(8):
                ts(C, "dve", idf[:, :, kt], be[:], 1024.0, float(kt * 128), ALU.mult, ALU.add, [idf], [be])
            ts(C, "dve", be[:], be[:], 31.0, None, ALU.min, None, [be], [be])
            for kt in range(8):
                ts(C, "dve", idf[:, :, kt], idf[:, :, kt], pid[:, 0:1], None, ALU.add, None, [idf], [idf, pid])
            cp(C, "dve", idw[:], idf[:], [idw], [idf])
            load_w(0)
            ts(C, "dve", oh[:], be[0:32, :], pid[0:32, 0:1], None, ALU.is_equal, None, [oh], [be, pid])
            cp(C, "dve", ohb16[:], oh[:], [ohb16], [oh])
            keys = [sb(C, st, [128, 32], F32, "key") for _ in range(2)]
            junks = [sb(C, st, [128, 32], F32, "junk") for _ in range(2)]
            d4s = [sb(C, st, [128, 4], F32, "d4") for _ in range(2)]
            ldx(0)
            ldx(1)
            for t in range(32):
                ldx(t + 2)
                x = xt[t % 4]
                xb = xb16[t % 2]
                key, junk, d4, mx8 = keys[t % 2], junks[t % 2], d4s[t % 2], mx8s[t % 2]
                act(C, xb[:], x[:], AF.Copy, [xb], [x])
                tt(C, "dve", key[:], rank_all[:, t, :], pstart[:], ALU.add, [key], [rank_all, pstart])
                stt(C, key[:], key[:], 1.0, msk_all[:, t, :], ALU.add, ALU.mult, [key], [key, msk_all])
                K.op("dve", lambda e: e.max(out=mx8[:], in_=key[:]), outs=[mx8], ins=[key])
                ts(C, "dve", d4[:], mx8[:, 0:4], -1.0, None, ALU.add, None, [d4], [mx8])
                cp(C, "dve", idx_all[:, t, :], d4[:], [idx_all], [d4])
                for k in range(4):
                    K.op("dve", lambda e: e.scalar_tensor_tensor(out=junk[:], in0=key[:], scalar=mx8[:, k:k + 1],
                                                                 in1=gates_all[:, t, :], op0=ALU.is_equal, op1=ALU.mult,
                                                                 accum_out=gsel_all[:, t, k:k + 1]),
                         outs=[junk, gsel_all], ins=[key, mx8, gates_all])
                for k in range(4):
                    K.dmaf("pool", lambda e: e.indirect_dma_start(out=Xs[:, :], out_offset=IOA(ap=idx_all[:, t, k:k + 1].bitcast(U32), axis=0),
                                                                  in_=xb[:, :], in_offset=None), ins=[xb, idx_all])
            K.barrier()
        with ExitStack() as st:
            xs_t = [sb(C, st, [128, 1024], BF16, "xs") for _ in range(8)]
            xT = [sb(C, st, [128, 8, 512], BF16, "xT") for _ in range(2)]
            actT = sb(C, st, [128, 8, 512], BF16, "actT")
            actTt = [Tile(actT.ap) for _ in range(8)]
            ev = [dict(g=sb(C, st, [128, 512], F32, "evg"), s=sb(C, st, [128, 512], F32, "evs"),
                       l=sb(C, st, [128, 512], F32, "evl"), t=sb(C, st, [128, 512], F32, "evt")) for _ in range(2)]
            yout = [sb(C, st, [128, 1024], BF16, "yout") for _ in range(3)]
            bsel = [sb(C, st, [128, 16], F32, "bsel") for _ in range(2)]

            def make_bsel(b):
                bank = C.PS[4 + b % 2]
                for ft in range(16):
                    mm(C, bank[:, ft:ft + 1], bupb[:, ft * 128:(ft + 1) * 128], ohb16[:, b:b + 1], True, True,
                       [bank], [bupb, ohb16], inc=(ft == 15))
                cp(C, "dve", bsel[b % 2][:], bank[:, 0:16], [bsel[b % 2]], [bank])

            def load_x(b):
                for i in range(4):
                    xs = xs_t[(b % 2) * 4 + i]
                    r0 = b * BSZ + i * 128
                    K.dma("sp", xs[:], Xs[r0:r0 + 128, :], outs=[xs])

            def transpose_x(b):
                xT_ = xT[b % 2]
                for i in range(4):
                    xs = xs_t[(b % 2) * 4 + i]
                    bank = C.PS[6 + i % 2]
                    psb = bank[:].bitcast(BF16)
                    for kt in range(8):
                        K.op("pe", lambda e: e.transpose(out=psb[:, kt * 128:(kt + 1) * 128], in_=xs[:, kt * 128:(kt + 1) * 128],
                                                         identity=identb[:]), outs=[bank], ins=[xs, identb], inc=(kt == 7))
                    cp(C, "act" if i % 2 else "dve", xT_[:, :, i * 128:(i + 1) * 128],
                       psb.rearrange("p (k n) -> p k n", k=8), [xT_], [bank])

            load_x(0)
            transpose_x(0)
            nev = 0
            nd = 0
            ny = 0
            for b in range(NBLK):
                if b + 1 < NBLK:
                    load_w(b + 1)
                    load_x(b + 1)
                wu = ringU[b % 2]
                wd = ringD[b % 2]
                xT_ = xT[b % 2]
                make_bsel(b)
                bs_ = bsel[b % 2]
                for j in range(8):
                    E = ev[nev % 2]
                    pg = C.PS[(nev % 2) * 2]
                    pl = C.PS[(nev % 2) * 2 + 1]
                    nev += 1
                    for kt in range(8):
                        mm(C, pg[:], wu[:, kt, j * 128:(j + 1) * 128], xT_[:, kt, :], kt == 0, kt == 7, [pg],
                           [ringUt[b % 2][kt], xT_], inc=(kt == 7))
                    for kt in range(8):
                        mm(C, pl[:], wu[:, kt, 1024 + j * 128:1024 + (j + 1) * 128], xT_[:, kt, :], kt == 0, kt == 7,
                           [pl], [ringUt[b % 2][kt], xT_], inc=(kt == 7))
                    ts(C, "dve", E["g"][:], pg[:], bs_[:, j:j + 1], 7.0, ALU.add, ALU.min, [E["g"]], [pg, bs_])
                    act(C, E["s"][:], E["g"][:], AF.Sigmoid, [E["s"]], [E["g"]], scale=1.702)
                    act(C, E["l"][:], pl[:], AF.Identity, [E["l"]], [pl, bs_], bias=bs_[:, 8 + j:9 + j])
                    ts(C, "dve", E["l"][:], E["l"][:], 7.0, -7.0, ALU.min, ALU.max, [E["l"]], [E["l"]])
                    tt(C, "dve", E["t"][:], E["g"][:], E["s"][:], ALU.mult, [E["t"]], [E["g"], E["s"]])
                    stt(C, actT[:, j, :], E["l"][:], 1.0, E["t"][:], ALU.add, ALU.mult, [actTt[j]], [E["l"], E["t"]])
                if b + 1 < NBLK:
                    transpose_x(b + 1)
                for i in range(4):
                    yo = yout[ny % 3]
                    ny += 1
                    for half in range(2):
                        ps = C.PS[4 + nd % 2]
                        nd += 1
                        for ft in range(8):
                            mm(C, ps[:], actT[:, ft, i * 128:(i + 1) * 128], wd[:, ft, half * 512:(half + 1) * 512], ft == 0,
                               ft == 7, [ps], [actTt[ft], ringDt[b % 2][ft]], inc=(ft == 7))
                        act(C, yo[:, half * 512:(half + 1) * 512], ps[:], AF.Copy, [yo], [ps])
                    r0 = b * BSZ + i * 128
                    K.dma("sp", Ys[r0:r0 + 128, :], yo[:], ins=[yo])
            K.barrier()
        with ExitStack() as st:
            g3 = sb(C, st, [128, 1024], F32, "g3")
            b3 = sb(C, st, [128, 1024], F32, "b3")
            load_bc(C, g3, P[L + "ln3_g"])
            load_bc(C, b3, P[L + "ln3_b"])
            xt = [sb(C, st, [128, 1024], F32, "xt") for _ in range(4)]
            yk = [sb(C, st, [128, 1024], BF16, "yk") for _ in range(12)]

            def ldx3(t):
                if t < 32:
                    K.dma("sp", xt[t % 4][:], src[t * 128:(t + 1) * 128, :], outs=[xt[t % 4]])
            yacc = [sb(C, st, [128, 1024], F32, "yacc") for _ in range(2)]
            LTs = [LNTmp(C, st) for _ in range(2)]
            ot = [sb(C, st, [128, 1024], F32, "ot") for _ in range(2)]

            def gather(t):
                for k in range(4):
                    y_ = yk[(t % 3) * 4 + k]
                    K.dmaf("pool", lambda e: e.indirect_dma_start(out=y_[:, :], out_offset=None, in_=Ys[:, :],
                                                                  in_offset=IOA(ap=idx_all[:, t, k:k + 1].bitcast(U32), axis=0)),
                           outs=[y_], ins=[idx_all])

            def cstage1(t):
                x = xt[t % 4]
                ya = yacc[t % 2]
                act(C, ya[:], x[:], AF.Copy, [ya], [x], scale=ALPHA)
                for half in range(2):
                    psb_ = C.PS[(t % 2) * 2 + half]
                    mm(C, psb_[:], gT_all[:, t, :], bdnb[:, half * 512:(half + 1) * 512], True, True, [psb_], [gT_all, bdnb])
                    tt(C, "dve", ya[:, half * 512:(half + 1) * 512], ya[:, half * 512:(half + 1) * 512], psb_[:], ALU.add,
                       [ya], [ya, psb_])
                for k in range(4):
                    y_ = yk[(t % 3) * 4 + k]
                    stt(C, ya[:], y_[:], gsel_all[:, t, k:k + 1], ya[:], ALU.mult, ALU.add, [ya], [y_, gsel_all, ya])

            def cstage2(t):
                ya = yacc[t % 2]
                o = ot[t % 2]
                layernorm(C, ya, o, LTs[t % 2], g3, b3, rs_eng="act")
                K.dma("sp", dst[t * 128:(t + 1) * 128, :], o[:], ins=[o])

            gather(0)
            gather(1)
            ldx3(0)
            ldx3(1)
            ldx3(2)
            cstage1(0)
            for t in range(32):
                ldx3(t + 3)
                if t + 2 < 32:
                    gather(t + 2)
                if t + 1 < 32:
                    cstage1(t + 1)
                cstage2(t)
            K.barrier()


def load_T(C, st, dst_ap, dst_tile, src_view, rows):
    K = C.K
    tmp = sb(C, st, [32, 128], F32, "ltT")
    K.dma("sp", tmp[0:rows, :], src_view, outs=[tmp])
    ps = C.PS[7]
    K.op("pe", lambda e: e.transpose(out=ps[:, 0:rows], in_=tmp[0:rows, :], identity=C.ident[0:rows, 0:rows]),
         outs=[ps], ins=[tmp, C.ident])
    cp(C, "dve", dst_ap, ps[:, 0:rows], [dst_tile], [ps])


def sincos(C, th_ap, th_tiles, n, sin_ap, sin_tiles, cos_ap, cos_tiles, ta, tb, tki, thr_ap=None, thr_tiles=()):
    a, b, ki = ta[:, 0:n], tb[:, 0:n], tki[:, 0:n]
    ts(C, "dve", a, th_ap, 1.0 / (2 * PI), None, ALU.mult, None, [ta], list(th_tiles))
    cp(C, "dve", ki, a, [tki], [ta])
    cp(C, "dve", a, ki, [ta], [tki])
    stt(C, b, a, -2 * PI, th_ap, ALU.mult, ALU.add, [tb], [ta] + list(th_tiles))
    if thr_ap is not None:
        cp(C, "dve", thr_ap, b, list(thr_tiles), [tb])
    ts(C, "dve", a, b, PI_LO, -PI_LO, ALU.min, ALU.max, [ta], [tb])
    act(C, sin_ap, a, AF.Sin, list(sin_tiles), [ta])
    ts(C, "dve", a, b, PI / 2, -2 * PI, ALU.is_gt, ALU.mult, [ta], [tb])
    stt(C, a, b, PI / 2, a, ALU.add, ALU.add, [ta], [ta, tb])
    ts(C, "dve", a, a, PI_LO, -PI_LO, ALU.min, ALU.max, [ta], [ta])
    act(C, cos_ap, a, AF.Sin, list(cos_tiles), [ta])


def phase_s5(C, src, dst):
    K = C.K
    P = C.P
    with ExitStack() as st5:
        Bw_re = sb(C, st5, [128, 32, 128], BF16, "Bwre")
        Bw_im = sb(C, st5, [128, 32, 128], BF16, "Bwim")
        Cw_re = sb(C, st5, [128, 32, 128], BF16, "Cwre")
        Cw_in = sb(C, st5, [128, 32, 128], BF16, "Cwin")
        thr = sb(C, st5, [128, 32], F32, "thr")
        mag = sb(C, st5, [128, 32], F32, "mag")
        dsk = sb(C, st5, [128, 8], F32, "dsk")
        iot = sb(C, st5, [128, 513], F32, "iot")
        K.op("pool", lambda e: e.iota(iot[:], pattern=[[1, 513]], base=0, channel_multiplier=0,
                                      allow_small_or_imprecise_dtypes=True), outs=[iot])
        with ExitStack() as st:
            lr = sb(C, st, [128, 32], F32, "lr")
            li = sb(C, st, [128, 32], F32, "li")
            ldt = sb(C, st, [128, 32], F32, "ldt")
            load_T(C, st, lr[:], lr, P["l1_lambda_re"].rearrange("(m gl) p -> m (gl p)", gl=2), 32)
            load_T(C, st, li[:], li, P["l1_lambda_im"].rearrange("(m gl) p -> m (gl p)", gl=2), 32)
            load_T(C, st, dsk[:], dsk, P["l1_d_skip"].rearrange("(ct gq) c -> ct (gq c)", gq=8), 8)
            ld32 = sb(C, st, [32, 2], F32, "ld32")
            K.dma("sp", ld32[:], P["l1_log_dt"].rearrange("(m gl) -> m gl", gl=2), outs=[ld32])
            ldx = sb(C, st, [32, 128], F32, "ldx")
            for gl in range(2):
                ts(C, "dve", ldx[:, gl * 64:(gl + 1) * 64], C.ones_f[0:32, 0:64], ld32[:, gl:gl + 1], None, ALU.mult, None,
                   [ldx], [C.ones_f, ld32])
            ps = C.PS[7]
            K.op("pe", lambda e: e.transpose(out=ps[:, 0:32], in_=ldx[:, :], identity=C.ident[0:32, 0:32]),
                 outs=[ps], ins=[ldx, C.ident])
            cp(C, "dve", ldt[:], ps[:, 0:32], [ldt], [ps])
            dt = sb(C, st, [128, 32], F32, "dt")
            act(C, dt[:], ldt[:], AF.Exp, [dt], [ldt])
            tmp = sb(C, st, [128, 32], F32, "tmp")
            th = sb(C, st, [128, 32], F32, "th")
            tt(C, "dve", tmp[:], lr[:], dt[:], ALU.mult, [tmp], [lr, dt])
            act(C, mag[:], tmp[:], AF.Exp, [mag], [tmp])
            tt(C, "dve", th[:], li[:], dt[:], ALU.mult, [th], [li, dt])
            sn = sb(C, st, [128, 32], F32, "sn")
            cs = sb(C, st, [128, 32], F32, "cs")
            ta = sb(C, st, [128, 32], F32, "ta")
            tb = sb(C, st, [128, 32], F32, "tb")
            tki = sb(C, st, [128, 32], I32, "tki")
            sincos(C, th[:], [th], 32, sn[:], [sn], cs[:], [cs], ta, tb, tki, thr_ap=thr[:], thr_tiles=[thr])
            ar1 = sb(C, st, [128, 32], F32, "ar1")
            ai = sb(C, st, [128, 32], F32, "ai")
            tt(C, "dve", ar1[:], mag[:], cs[:], ALU.mult, [ar1], [mag, cs])
            ts(C, "dve", ar1[:], ar1[:], -1.0, None, ALU.add, None, [ar1], [ar1])
            tt(C, "dve", ai[:], mag[:], sn[:], ALU.mult, [ai], [mag, sn])
            den = sb(C, st, [128, 32], F32, "den")
            t2 = sb(C, st, [128, 32], F32, "t2")
            tt(C, "dve", den[:], lr[:], lr[:], ALU.mult, [den], [lr])
            tt(C, "dve", t2[:], li[:], li[:], ALU.mult, [t2], [li])
            tt(C, "dve", den[:], den[:], t2[:], ALU.add, [den], [den, t2])
            K.op("dve", lambda e: e.reciprocal(den[:], den[:]), outs=[den], ins=[den])
            zr = sb(C, st, [128, 32], F32, "zr")
            zi = sb(C, st, [128, 32], F32, "zi")
            nzi = sb(C, st, [128, 32], F32, "nzi")
            tt(C, "dve", zr[:], ar1[:], lr[:], ALU.mult, [zr], [ar1, lr])
            tt(C, "dve", t2[:], ai[:], li[:], ALU.mult, [t2], [ai, li])
            tt(C, "dve", zr[:], zr[:], t2[:], ALU.add, [zr], [zr, t2])
            tt(C, "dve", zr[:], zr[:], den[:], ALU.mult, [zr], [zr, den])
            tt(C, "dve", zi[:], ai[:], lr[:], ALU.mult, [zi], [ai, lr])
            tt(C, "dve", t2[:], ar1[:], li[:], ALU.mult, [t2], [ar1, li])
            tt(C, "dve", zi[:], zi[:], t2[:], ALU.subtract, [zi], [zi, t2])
            tt(C, "dve", zi[:], zi[:], den[:], ALU.mult, [zi], [zi, den])
            ts(C, "dve", nzi[:], zi[:], -1.0, None, ALU.mult, None, [nzi], [zi])
            Bre = sb(C, st, [128, 32, 16], F32, "Bre")
            Bim = sb(C, st, [128, 32, 16], F32, "Bim")
            K.dma("sp", Bre[:], P["l1_b_re"].rearrange("(m gl) p c -> (gl p) m c", gl=2), outs=[Bre])
            K.dma("sp", Bim[:], P["l1_b_im"].rearrange("(m gl) p c -> (gl p) m c", gl=2), outs=[Bim])
            bbr = sb(C, st, [128, 32, 16], F32, "bbr")
            bbi = sb(C, st, [128, 32, 16], F32, "bbi")
            for m in range(32):
                ts(C, "dve", bbr[:, m, :], Bre[:, m, :], zr[:, m:m + 1], None, ALU.mult, None, [bbr], [Bre, zr])
                stt(C, bbr[:, m, :], Bim[:, m, :], nzi[:, m:m + 1], bbr[:, m, :], ALU.mult, ALU.add, [bbr], [Bim, nzi, bbr])
                ts(C, "dve", bbi[:, m, :], Bim[:, m, :], zr[:, m:m + 1], None, ALU.mult, None, [bbi], [Bim, zr])
                stt(C, bbi[:, m, :], Bre[:, m, :], zi[:, m:m + 1], bbi[:, m, :], ALU.mult, ALU.add, [bbi], [Bre, zi, bbi])
            in3 = sb(C, st, [128, 32, 128], F32, "in3")
            K.op("pool", lambda e: e.memset(in3[:], 0.0), outs=[in3])
            for (bb, Bw) in ((bbr, Bw_re), (bbi, Bw_im)):
                for m in range(32):
                    for gl in range(2):
                        c0 = (2 * (m % 4) + gl) * 16
                        cp(C, "dve" if gl else "pool", in3[gl * 64:(gl + 1) * 64, m, c0:c0 + 16],
                           bb[gl * 64:(gl + 1) * 64, m, :], [in3], [bb])
                for m4 in range(8):
                    ps = C.PS[m4 % 2]
                    for j in range(4):
                        m = m4 * 4 + j
                        K.op("pe", lambda e: e.transpose(out=ps[:, j * 128:(j + 1) * 128], in_=in3[:, m, :],
                                                         identity=C.ident[:]), outs=[ps], ins=[in3, C.ident], inc=(j == 3))
                    cp(C, "act" if m4 % 2 else "dve", Bw[:, m4 * 4:(m4 + 1) * 4, :],
                       ps[:].rearrange("p (j n) -> p j n", j=4), [Bw], [ps])
            mask2 = sb(C, st, [128, 128], F32, "mask2")
            for a in range(4):
                K.op("pool", lambda e: e.affine_select(out=mask2[a * 32:(a + 1) * 32, 0:64], in_=C.ones_f[a * 32:(a + 1) * 32, 0:64],
                                                       pattern=[[0, 64]], compare_op=ALU.is_ge, fill=0.0, base=15,
                                                       channel_multiplier=-1), outs=[mask2], ins=[C.ones_f])
                K.op("pool", lambda e: e.affine_select(out=mask2[a * 32:(a + 1) * 32, 64:128], in_=C.ones_f[a * 32:(a + 1) * 32, 0:64],
                                                       pattern=[[0, 64]], compare_op=ALU.is_ge, fill=0.0, base=-16,
                                                       channel_multiplier=1), outs=[mask2], ins=[C.ones_f])
            Cre = sb(C, st, [128, 8, 64], F32, "Cre")
            Cim = sb(C, st, [128, 8, 64], F32, "Cim")
            K.dma("sp", Cre[:], P["l1_c_re"].rearrange("(ct gq) c p -> (gq c) ct p", gq=8), outs=[Cre])
            K.dma("sp", Cim[:], P["l1_c_im"].rearrange("(ct gq) c p -> (gq c) ct p", gq=8), outs=[Cim])
            in4 = sb(C, st, [128, 4, 128], F32, "in4")
            K.op("pool", lambda e: e.memset(in4[:], 0.0), outs=[in4])
            ni = 0
            for (Cc, Cw, sc) in ((Cre, Cw_re, 1.0), (Cim, Cw_in, -1.0)):
                for ct in range(8):
                    for j4 in range(4):
                        for gl in range(2):
                            tt(C, "dve" if gl else "pool", in4[j4 * 32:(j4 + 1) * 32, j4, gl * 64:(gl + 1) * 64],
                               Cc[j4 * 32:(j4 + 1) * 32, ct, :], mask2[j4 * 32:(j4 + 1) * 32, gl * 64:(gl + 1) * 64],
                               ALU.mult, [in4], [Cc, mask2])
                    ps = C.PS[2 + ni % 2]
                    ni += 1
                    for j4 in range(4):
                        K.op("pe", lambda e: e.transpose(out=ps[:, j4 * 128:(j4 + 1) * 128], in_=in4[:, j4, :],
                                                         identity=C.ident[:]), outs=[ps], ins=[in4, C.ident], inc=(j4 == 3))
                    act(C, Cw[:, ct * 4:(ct + 1) * 4, :], ps[:].rearrange("p (j n) -> p j n", j=4), AF.Copy, [Cw], [ps],
                        scale=sc)
            K.barrier()

        for s in range(2):
            with ExitStack() as sts:
                uTa = sb(C, sts, [128, 8, 2048], BF16, "uT")
                yga = sb(C, sts, [128, 8, 2048], BF16, "yg")
                uT = [Tile(uTa.ap) for _ in range(4)]
                yg = [Tile(yga.ap) for _ in range(4)]
                with ExitStack() as st:
                    w1 = sb(C, st, [128, 8, 1024], BF16, "w1in")
                    load_wbf(C, w1, P["l1_w_in"], 1024)
                    xt = [sb(C, st, [128, 1024], F32, "xt") for _ in range(3)]
                    xTb = [sb(C, st, [128, 8, 512], BF16, "xTb") for _ in range(2)]
                    nx = 0
                    for blk in range(4):
                        xb_ = xTb[blk % 2]
                        for t4 in range(4):
                            x = xt[nx % 3]
                            nx += 1
                            r0 = s * SEQ + blk * 512 + t4 * 128
                            K.dma("sp", x[:], src[r0:r0 + 128, :], outs=[x])
                            transpose8(C, x, xb_, lambda k0: xb_[:, k0:k0 + 4, t4 * 128:(t4 + 1) * 128], C.PS[0], C.PS[1])
                        for ft in range(8):
                            ps = C.PS[2 + ft % 4]
                            for kt in range(8):
                                mm(C, ps[:], w1[:, kt, ft * 128:(ft + 1) * 128], xb_[:, kt, :], kt == 0, kt == 7,
                                   [ps], [w1, xb_])
                            cp(C, "act" if ft % 2 else "dve", uTa[:, ft, blk * 512:(blk + 1) * 512], ps[:], [uT[blk]], [ps])
                    K.barrier()
                with ExitStack() as st:
                    cst = sb(C, st, [128, 4, 513], F32, "cst")
                    snt = sb(C, st, [128, 4, 513], F32, "snt")
                    nsl = sb(C, st, [128, 4], F32, "nsl")
                    rfull = sb(C, st, [128, 4, 512], F32, "rfull")
                    tht = sb(C, st, [128, 513], F32, "tht")
                    ta = sb(C, st, [128, 513], F32, "ta")
                    tb = sb(C, st, [128, 513], F32, "tb")
                    tki = sb(C, st, [128, 513], I32, "tki")
                    car = [sb(C, st, [128, 2], F32, "car") for _ in range(4)]
                    sets = []
                    for _k in range(3):
                        sets.append(dict(
                            br=sb(C, st, [128, 512], F32, "br"), bi=sb(C, st, [128, 512], F32, "bi"),
                            tq=[sb(C, st, [128, 512], F32, "tq") for _ in range(4)],
                            zin=None,
                            zz=[sb(C, st, [128, 512], F32, "zz") for _ in range(2)],
                            xr=sb(C, st, [128, 512], BF16, "xr"), xi=sb(C, st, [128, 512], BF16, "xi"),
                            cart=sb(C, st, [128, 2], F32, "cart")))
                    GT = GeluTmp(C, st, 512)
                    nit = 0
                    for ct in range(8):
                        for j4 in range(4):
                            m = ct * 4 + j4
                            ts(C, "dve", tht[:], iot[:], thr[:, m:m + 1], None, ALU.mult, None, [tht], [iot, thr])
                            sincos(C, tht[:], [tht], 513, snt[:, j4, :], [snt], cst[:, j4, :], [cst], ta, tb, tki)
                            ts(C, "dve", nsl[:, j4:j4 + 1], snt[:, j4, 512:513], -1.0, None, ALU.mult, None, [nsl], [snt])
                            ts(C, "pool", rfull[:, j4, :], iot[:, 0:512], 0.0, mag[:, m:m + 1], ALU.mult, ALU.add,
                               [rfull], [iot, mag])
                            K.op("pool", lambda e: e.memset(car[j4][:], 0.0), outs=[car[j4]])
                        its = [(c, j4) for c in range(4) for j4 in range(4)]

                        def setof(n):
                            S_ = sets[n % 3]
                            return (S_["br"], S_["bi"], S_["tq"], S_["zin"], S_["zz"], S_["xr"], S_["xi"], S_["cart"])

                        def stB(n, c, j4):
                            m = ct * 4 + j4
                            pr = C.PS[0]
                            pi_ = C.PS[1]
                            br, bi, tq, zin, zz, xr, xi, cart = setof(n)
                            rhs = uTa[:, ct, c * 512:(c + 1) * 512]
                            mm(C, pr[:], Bw_re[:, m, :], rhs, True, True, [pr], [Bw_re, uT[c]])
                            mm(C, pi_[:], Bw_im[:, m, :], rhs, True, True, [pi_], [Bw_im, uT[c]])
                            act(C, br[:], pr[:], AF.Copy, [br], [pr])
                            act(C, bi[:], pi_[:], AF.Copy, [bi], [pi_])

                        def stR1(n, c, j4):
                            br, bi, tq, zin, zz, xr, xi, cart = setof(n)
                            cs_ = cst[:, j4, 0:512]
                            sn_ = snt[:, j4, 0:512]
                            tt(C, "dve", tq[0][:], br[:], cs_, ALU.mult, [tq[0]], [br, cst])
                            tt(C, "pool", tq[1][:], bi[:], sn_, ALU.mult, [tq[1]], [bi, snt])
                            tt(C, "dve", tq[2][:], bi[:], cs_, ALU.mult, [tq[2]], [bi, cst])
                            tt(C, "pool", tq[3][:], br[:], sn_, ALU.mult, [tq[3]], [br, snt])
                            mm(C, C.PS[2][:], C.ident[:], tq[0][:], True, False, [C.PS[2]], [C.ident, tq[0]], inc=True)
                            mm(C, C.PS[2][:], C.ident[:], tq[1][:], False, True, [C.PS[2]], [C.ident, tq[1]], inc=True)
                            mm(C, C.PS[3][:], C.ident[:], tq[2][:], True, False, [C.PS[3]], [C.ident, tq[2]], inc=True)
                            mm(C, C.PS[3][:], C.nident[:], tq[3][:], False, True, [C.PS[3]], [C.nident, tq[3]], inc=True)

                        def stR2(n, c, j4):
                            br, bi, tq, zin, zz, xr, xi, cart = setof(n)
                            for k in range(2):
                                K.op("dve", lambda e: e.tensor_tensor_scan(out=zz[k][:], data0=rfull[:, j4, :],
                                                                           data1=C.PS[2 + k][:], initial=car[j4][:, k:k + 1],
                                                                           op0=ALU.mult, op1=ALU.add),
                                     outs=[zz[k]], ins=[rfull, C.PS[2 + k], car[j4]])
                            c5 = cst[:, j4, 512:513]
                            s5 = snt[:, j4, 512:513]
                            act(C, cart[:, 0:1], zz[0][:, 511:512], AF.Copy, [cart], [zz[0], cst], scale=c5)
                            act(C, cart[:, 1:2], zz[0][:, 511:512], AF.Copy, [cart], [zz[0], snt], scale=s5)
                            act(C, car[j4][:, 0:1], zz[1][:, 511:512], AF.Identity, [car[j4]], [zz[1], nsl, cart],
                                scale=nsl[:, j4:j4 + 1], bias=cart[:, 0:1])
                            act(C, car[j4][:, 1:2], zz[1][:, 511:512], AF.Identity, [car[j4]], [zz[1], cst, cart],
                                scale=c5, bias=cart[:, 1:2])

                        def stR3C(n, c, j4):
                            m = ct * 4 + j4
                            br, bi, tq, zin, zz, xr, xi, cart = setof(n)
                            cs_ = cst[:, j4, 0:512]
                            sn_ = snt[:, j4, 0:512]
                            yps = C.PS[4 + (ct * 4 + c) % 2]
                            tt(C, "dve", tq[0][:], zz[0][:], cs_, ALU.mult, [tq[0]], [zz[0], cst])
                            tt(C, "pool", tq[1][:], zz[1][:], sn_, ALU.mult, [tq[1]], [zz[1], snt])
                            tt(C, "dve", tq[3][:], zz[1][:], cs_, ALU.mult, [tq[3]], [zz[1], cst])
                            tt(C, "pool", tq[2][:], zz[0][:], sn_, ALU.mult, [tq[2]], [zz[0], snt])
                            mm(C, C.PS[6][:], C.ident[:], tq[0][:], True, False, [C.PS[6]], [C.ident, tq[0]], inc=True)
                            mm(C, C.PS[6][:], C.nident[:], tq[1][:], False, True, [C.PS[6]], [C.nident, tq[1]], inc=True)
                            act(C, xr[:], C.PS[6][:], AF.Copy, [xr], [C.PS[6]])
                            tt(C, "pool", xi[:], tq[2][:], tq[3][:], ALU.add, [xi], [tq[2], tq[3]])
                            mm(C, yps[:], Cw_re[:, m, :], xr[:], j4 == 0, False, [yps], [Cw_re, xr], inc=True)
                            mm(C, yps[:], Cw_in[:, m, :], xi[:], False, j4 == 3, [yps], [Cw_in, xi], inc=True)
                            if j4 == 3:
                                stt(C, GT.xs[:], uTa[:, ct, c * 512:(c + 1) * 512], dsk[:, ct:ct + 1], yps[:], ALU.mult,
                                    ALU.add, [GT.xs], [uT[c], dsk, yps])
                                gelu(C, None, None, yga[:, ct, c * 512:(c + 1) * 512], [yg[c]], GT, 512, xs_given=True)

                        stB(0, *its[0])
                        stR1(0, *its[0])
                        stB(1, *its[1])
                        for n, (c, j4) in enumerate(its):
                            if n + 2 < len(its):
                                stB(n + 2, *its[n + 2])
                            stR2(n, c, j4)
                            if n + 1 < len(its):
                                stR1(n + 1, *its[n + 1])
                            stR3C(n, c, j4)
                    K.barrier()
                with ExitStack() as st:
                    wv = sb(C, st, [128, 8, 1024], BF16, "wval")
                    wg = sb(C, st, [128, 8, 1024], BF16, "wgate")
                    load_wbf(C, wv, P["l1_w_val"], 1024)
                    load_wbf(C, wg, P["l1_w_gate"], 1024)
                    g1 = sb(C, st, [128, 1024], F32, "g1")
                    b1 = sb(C, st, [128, 1024], F32, "b1")
                    load_bc(C, g1, P["l1_ln1_g"])
                    load_bc(C, b1, P["l1_ln1_b"])
                    xt = [sb(C, st, [128, 1024], F32, "xt") for _ in range(3)]
                    sg = [sb(C, st, [128, 512], F32, "sgm") for _ in range(2)]
                    hv = [sb(C, st, [128, 512], F32, "hv") for _ in range(2)]
                    zs = [sb(C, st, [128, 1024], F32, "z") for _ in range(2)]
                    LTs = [LNTmp(C, st) for _ in range(2)]
                    ot = [sb(C, st, [128, 1024], F32, "ot") for _ in range(2)]

                    def ostage1(t16):
                        x = xt[t16 % 3]
                        z = zs[t16 % 2]
                        r0 = s * SEQ + t16 * 128
                        K.dma("sp", x[:], src[r0:r0 + 128, :], outs=[x])
                        for half in range(2):
                            pv = C.PS[(t16 % 2) * 4 + half]
                            pg = C.PS[(t16 % 2) * 4 + 2 + half]
                            for ft in range(8):
                                mm(C, pv[:], yga[:, ft, t16 * 128:(t16 + 1) * 128], wv[:, ft, half * 512:(half + 1) * 512],
                                   ft == 0, ft == 7, [pv], [yg[t16 // 4], wv])
                            for ft in range(8):
                                mm(C, pg[:], yga[:, ft, t16 * 128:(t16 + 1) * 128], wg[:, ft, half * 512:(half + 1) * 512],
                                   ft == 0, ft == 7, [pg], [yg[t16 // 4], wg])
                            act(C, sg[half][:], pg[:], AF.Sigmoid, [sg[half]], [pg])
                            tt(C, "dve", hv[half][:], pv[:], sg[half][:], ALU.mult, [hv[half]], [pv, sg[half]])
                            stt(C, z[:, half * 512:(half + 1) * 512], x[:, half * 512:(half + 1) * 512], ALPHA, hv[half][:],
                                ALU.mult, ALU.add, [z], [x, hv[half]])

                    def ostage2(t16):
                        r0 = s * SEQ + t16 * 128
                        o = ot[t16 % 2]
                        layernorm(C, zs[t16 % 2], o, LTs[t16 % 2], g1, b1)
                        K.dma("sp", dst[r0:r0 + 128, :], o[:], ins=[o])

                    ostage1(0)
                    for t16 in range(16):
                        if t16 + 1 < 16:
                            ostage1(t16 + 1)
                        ostage2(t16)
                    K.barrier()


def build(stop_after=None):
    nc = bass.Bass("TRN2", target_bir_lowering=False)
    C = Ctx()
    C.nc = nc
    C.n = 0
    P = {}
    P["x"] = nc.dram_tensor("x", [NTOK, D], F32, kind="ExternalInput").ap()
    P["mem"] = nc.dram_tensor("mem", [512, D], F32, kind="ExternalInput").ap()
    for k, shp in PARAM_SHAPES.items():
        P[k] = nc.dram_tensor(k, list(shp), F32, kind="ExternalInput").ap()
    out = nc.dram_tensor("out", [NTOK, D], F32, kind="ExternalOutput").ap()
    xa = nc.dram_tensor("xa", [NTOK, D], F32, kind="Internal").ap()
    xb = nc.dram_tensor("xb", [NTOK, D], F32, kind="Internal").ap()
    xc = nc.dram_tensor("xc", [NTOK, D], F32, kind="Internal").ap()
    Xs = nc.dram_tensor("Xs", [NBLK * BSZ, D], BF16, kind="Internal").ap()
    Ys = nc.dram_tensor("Ys", [NBLK * BSZ, D], BF16, kind="Internal").ap()
    C.P = P
    es = ExitStack()
    with es:
        C.es = es
        C.K = Sched(nc, es)
        C.PS = [Tile(es.enter_context(nc.psum_tensor("ps%d" % i, [128, 512], F32))) for i in range(8)]
        phase_consts(C)
        phase_kv(C)

        def final(src_ap):
            if src_ap is not out:
                C.K.dma("sp", out, src_ap)
            C.K.barrier()

        for s in range(2):
            with ExitStack() as st:
                dT = sb(C, st, [128, 4, 2048], BF16, "dT")
                phase_l0_attn(C, s, dT, P["x"])
                phase_l0_sgu_out(C, s, dT, P["x"], xa)
        if stop_after == "l0mix":
            final(xa)
            return nc
        phase_cross(C, "l0_", xa, xb)
        if stop_after == "l0cross":
            final(xb)
            return nc
        phase_moe_sparse(C, "l0_", xb, xc, Xs, Ys)
        if stop_after == "l0":
            final(xc)
            return nc
        phase_s5(C, xc, xa)
        if stop_after == "l1mix":
            final(xa)
            return nc
        phase_cross(C, "l1_", xa, xb)
        phase_moe_sparse(C, "l1_", xb, out, Xs, Ys)
    return nc


def make_in_maps(inputs):
    x = np.ascontiguousarray(inputs["x"], dtype=np.float32)
    mem = np.ascontiguousarray(inputs["mem"], dtype=np.float32)
    shared = {k: np.ascontiguousarray(inputs[k], dtype=np.float32) for k in PARAM_SHAPES}
    maps = []
    for c in range(NCORES):
        m = dict(shared)
        m["x"] = x[2 * c:2 * c + 2].reshape(NTOK, D)
        m["mem"] = mem[2 * c:2 * c + 2].reshape(512, D)
        maps.append(m)
    return maps


def kernel(**inputs):
    nc = build()
    maps = make_in_maps(inputs)
    res = run_bass_kernel_spmd(nc, maps, core_ids=list(range(NCORES)))
    outs = [np.asarray(r["out"]).reshape(2, SEQ, D) for r in res.results]
    return np.concatenate(outs, axis=0).astype(np.float32)
```

```python
import math
import numpy as np
from contextlib import ExitStack
import concourse.bass as bass
import concourse.mybir as mybir
from concourse.bass_utils import run_bass_kernel_spmd

F32 = mybir.dt.float32
BF16 = mybir.dt.bfloat16
I32 = mybir.dt.int32
ALU = mybir.AluOpType
AF = mybir.ActivationFunctionType
AX = mybir.AxisListType

SEM_ROLL = 24000
NDS = 14

NCORES = 8
NTOK = 4096
SEQ = 2048
D = 1024
ALPHA = 4.0 ** 0.25
EPS = 1e-5
LAM_INIT = 0.8 - 0.6 * math.exp(0.0)
PI = math.pi
PI_LO = 3.1415925

PARAM_SHAPES = dict(
    w_mem_kv=(1024, 2048), l0_w_in=(1024, 2560), l0_sgu_ln_g=(512,), l0_sgu_ln_b=(512,),
    l0_w_spatial=(4, 128, 128), l0_b_spatial=(4, 128), l0_lam_q1=(64,), l0_lam_k1=(64,),
    l0_lam_q2=(64,), l0_lam_k2=(64,), l0_subln_g=(128,), l0_w_out=(1024, 1024),
    l1_w_in=(1024, 1024), l1_log_dt=(64,), l1_lambda_re=(64, 64), l1_lambda_im=(64, 64),
    l1_b_re=(64, 64, 16), l1_b_im=(64, 64, 16), l1_c_re=(64, 16, 64), l1_c_im=(64, 16, 64),
    l1_d_skip=(64, 16), l1_w_val=(1024, 1024), l1_w_gate=(1024, 1024),
)
for _l in ("l0_", "l1_"):
    PARAM_SHAPES.update({
        _l + "ln1_g": (1024,), _l + "ln1_b": (1024,), _l + "xq": (1024, 1024), _l + "xo": (1024, 1024),
        _l + "ln2_g": (1024,), _l + "ln2_b": (1024,), _l + "router_w": (1024, 32), _l + "router_b": (32,),
        _l + "exp_w_up": (32, 1024, 2048), _l + "exp_b_up": (32, 2048), _l + "exp_w_down": (32, 1024, 1024),
        _l + "exp_b_down": (32, 1024), _l + "ln3_g": (1024,), _l + "ln3_b": (1024,),
    })


class Tile:
    __slots__ = ("ap", "w", "r", "name")

    def __init__(self, ap, name=""):
        self.ap = ap
        self.w = None
        self.r = []
        self.name = name

    def __getitem__(self, k):
        return self.ap[k]


class Eng:
    def __init__(self, K, name, obj):
        self.name = name
        self.obj = obj
        self.sem = K.newsem(name)
        self.count = 0
        self.waited = {}
        self.dsems = None
        self.di = 0


class Sched:
    def __init__(self, nc, es):
        self.nc = nc
        self.es = es
        self.nsem = 0
        self.E = {}
        for n, o in (("pe", nc.tensor), ("act", nc.scalar), ("dve", nc.vector),
                     ("pool", nc.gpsimd), ("sp", nc.sync)):
            self.E[n] = Eng(self, n, o)
        self.ninst = 0

    def newsem(self, name):
        self.nsem += 1
        return self.es.enter_context(self.nc.semaphore("s%s%d" % (name, self.nsem)))

    def _deps(self, outs, ins):
        deps = []
        for t in ins:
            if t.w is not None:
                if isinstance(t.w, list):
                    deps.extend(t.w)
                else:
                    deps.append(t.w)
        for t in outs:
            if t.w is not None:
                if isinstance(t.w, list):
                    deps.extend(t.w)
                else:
                    deps.append(t.w)
            deps.extend(t.r)
        return deps

    def _wait(self, eng, deps):
        e = self.E[eng]
        for (pe_, sem, val) in deps:
            if pe_ == "pe" and eng == "pe":
                continue
            key = sem.num
            if e.waited.get(key, 0) >= val:
                continue
            e.obj.wait_ge(sem, val)
            e.waited[key] = val
            self.ninst += 1

    def op(self, eng, fn, outs=(), ins=(), inc=True):
        e = self.E[eng]
        self._wait(eng, self._deps(outs, ins))
        inst = fn(e.obj)
        self.ninst += 1
        if e.count >= SEM_ROLL and inc:
            e.sem = self.newsem(eng)
            e.count = 0
        if inc:
            e.count += 1
            inst.then_inc(e.sem, 1)
            rec = (eng, e.sem, e.count)
        else:
            rec = (eng, e.sem, e.count + 1)
        for t in ins:
            t.r.append(rec)
        for t in outs:
            t.w = rec
            t.r = []
        return inst

    def dma(self, q, out_ap, in_ap, outs=(), ins=(), **kw):
        return self.dmaf(q, lambda e: e.dma_start(out=out_ap, in_=in_ap, **kw), outs=outs, ins=ins)

    def dmaf(self, q, fn, outs=(), ins=()):
        e = self.E[q]
        if e.dsems is None:
            e.dsems = [[self.newsem(q + "d"), 0] for _ in range(NDS)]
        self._wait(q, self._deps(outs, ins))
        slot = e.dsems[e.di % NDS]
        e.di += 1
        if slot[1] >= SEM_ROLL:
            slot[0] = self.newsem(q + "d")
            slot[1] = 0
        sem, tot = slot
        if tot > 0 and e.waited.get(sem.num, 0) < tot:
            e.obj.wait_ge(sem, tot)
            e.waited[sem.num] = tot
        inst = fn(e.obj)
        inst.then_inc(sem, 16)
        slot[1] = tot + 16
        rec = ("dma", sem, tot + 16)
        self.last_rec = rec
        self.ninst += 1
        for t in ins:
            t.r.append(rec)
        for t in outs:
            t.w = rec
            t.r = []
        return inst

    def pre(self, eng, ins):
        self._wait(eng, self._deps((), ins))

    def barrier(self):
        recs = []
        for n, e in self.E.items():
            if e.count > 0:
                recs.append((n + "_b", e.sem, e.count))
            if e.dsems:
                for sem, tot in e.dsems:
                    if tot > 0:
                        recs.append(("dma", sem, tot))
        for n in self.E:
            self._wait(n, recs)


class Ctx:
    pass


def sb(C, st, shape, dt=F32, name="t"):
    C.n += 1
    t = st.enter_context(C.nc.sbuf_tensor("%s_%d" % (name, C.n), list(shape), dt))
    return Tile(t, name)


def mm(C, out_ap, lhsT, rhs, start, stop, outs, ins, inc=None):
    if inc is None:
        inc = stop
    C.K.op("pe", lambda e: e.matmul(out_ap, lhsT, rhs, start=start, stop=stop), outs=outs, ins=ins, inc=inc)


def tt(C, eng, out_ap, a, b, op, outs, ins):
    C.K.op(eng, lambda e: e.tensor_tensor(out=out_ap, in0=a, in1=b, op=op), outs=outs, ins=ins)


def ts(C, eng, out_ap, a, s1, s2, op0, op1, outs, ins):
    if op1 is None:
        C.K.op(eng, lambda e: e.tensor_scalar(out=out_ap, in0=a, scalar1=s1, scalar2=None, op0=op0), outs=outs, ins=ins)
    else:
        C.K.op(eng, lambda e: e.tensor_scalar(out=out_ap, in0=a, scalar1=s1, scalar2=s2, op0=op0, op1=op1), outs=outs, ins=ins)


def stt(C, out_ap, a, scalar, b, op0, op1, outs, ins):
    C.K.op("dve", lambda e: e.scalar_tensor_tensor(out=out_ap, in0=a, scalar=scalar, in1=b, op0=op0, op1=op1),
           outs=outs, ins=ins)


def act(C, out_ap, in_ap, func, outs, ins, scale=1.0, bias=None):
    if bias is None:
        C.K.op("act", lambda e: e.activation(out=out_ap, in_=in_ap, func=func, scale=scale), outs=outs, ins=ins)
    else:
        C.K.op("act", lambda e: e.activation(out=out_ap, in_=in_ap, func=func, scale=scale, bias=bias), outs=outs, ins=ins)


def cp(C, eng, out_ap, in_ap, outs, ins):
    if eng == "act":
        act(C, out_ap, in_ap, AF.Copy, outs, ins)
    else:
        C.K.op(eng, lambda e: e.tensor_copy(out_ap, in_ap), outs=outs, ins=ins)


def load_wbf(C, dst, src2d, ncols, col0=0, kts=8):
    c = 0
    recs = []
    first = True
    while c < ncols:
        n = min(512, ncols - c)
        C.K.dma("pool", dst[:, :, c:c + n],
                src2d[:, col0 + c:col0 + c + n].rearrange("(kt p) n -> p kt n", p=128), outs=([dst] if first else []))
        recs.append(C.K.last_rec)
        first = False
        c += n
    dst.w = recs
    dst.r = []


def load_bc(C, dst, vec):
    C.K.dma("sp", dst[:], vec.partition_broadcast(128), outs=[dst])


def transpose8(C, src, dst, dst_ap_fn, psA, psB, eng0="dve", eng1="act", extra=None):
    for kt in range(8):
        bank = psA if kt < 4 else psB
        C.K.op("pe", lambda e: e.transpose(out=bank[:, (kt % 4) * 128:(kt % 4 + 1) * 128],
                                           in_=src[:, kt * 128:(kt + 1) * 128], identity=C.ident[:]),
               outs=[bank], ins=[src, C.ident], inc=(kt % 4 == 3))
    cp(C, eng0, dst_ap_fn(0), psA[:].rearrange("p (k n) -> p k n", k=4), [dst], [psA])
    if extra is not None:
        extra(0, psA)
    cp(C, eng1, dst_ap_fn(4), psB[:].rearrange("p (k n) -> p k n", k=4), [dst], [psB])
    if extra is not None:
        extra(4, psB)


class LNTmp:
    def __init__(self, C, st):
        self.st6 = sb(C, st, [128, 2, 6], F32, "lnst")
        self.mv = sb(C, st, [128, 2], F32, "lnmv")
        self.vp = sb(C, st, [128, 1], F32, "lnvp")
        self.rs = sb(C, st, [128, 1], F32, "lnrs")
        self.zn = sb(C, st, [128, 1024], F32, "lnzn")


def layernorm(C, z, out, T, g_bc, b_bc, rs_eng="act"):
    K = C.K
    for c in range(2):
        K.op("dve", lambda e: e.bn_stats(out=T.st6[:, c, :], in_=z[:, c * 512:(c + 1) * 512]), outs=[T.st6], ins=[z])
    K.op("dve", lambda e: e.bn_aggr(out=T.mv[:], in_=T.st6[:].rearrange("p a b -> p (a b)")), outs=[T.mv], ins=[T.st6])
    ts(C, "dve", T.vp[:], T.mv[:, 1:2], EPS, None, ALU.add, None, [T.vp], [T.mv])
    if rs_eng == "act":
        act(C, T.rs[:], T.vp[:], AF.Ln, [T.rs], [T.vp])
        act(C, T.rs[:], T.rs[:], AF.Exp, [T.rs], [T.rs], scale=-0.5)
    else:
        tt(C, "pool", T.rs[:], T.vp[:], C.neghalf[:, 0:1], ALU.pow, [T.rs], [T.vp, C.neghalf])
    stt(C, T.zn[:], z[:], T.mv[:, 0:1], g_bc[:], ALU.subtract, ALU.mult, [T.zn], [z, T.mv, g_bc])
    stt(C, out[:], T.zn[:], T.rs[:, 0:1], b_bc[:], ALU.mult, ALU.add, [out], [T.zn, T.rs, b_bc])


class GeluTmp:
    def __init__(self, C, st, n):
        self.xs = sb(C, st, [128, n], F32, "gxs")


def gelu(C, src_ap, src_tiles, out_ap, out_tiles, T, n, xs_given=False):
    if xs_given:
        act(C, out_ap, T.xs[:, 0:n], AF.Gelu_apprx_tanh, out_tiles, [T.xs])
    else:
        act(C, out_ap, src_ap, AF.Gelu_apprx_tanh, out_tiles, src_tiles)


def phase_consts(C):
    K = C.K
    st = C.es
    C.ident = sb(C, st, [128, 128], F32, "ident")
    C.ones_f = sb(C, st, [128, 128], F32, "onesf")
    C.ones_b = sb(C, st, [128, 128], BF16, "onesb")
    C.neghalf = sb(C, st, [128, 512], F32, "neghalf")
    C.nident = sb(C, st, [128, 128], F32, "nident")
    C.kmT = sb(C, st, [128, 2, 8, 256], BF16, "kmT")
    C.vmem = sb(C, st, [128, 2, 2, 1024], BF16, "vmem")
    K.op("pool", lambda e: e.memset(C.ones_f[:], 1.0), outs=[C.ones_f])
    K.op("pool", lambda e: e.memset(C.ones_b[:], 1.0), outs=[C.ones_b])
    K.op("pool", lambda e: e.memset(C.neghalf[:], -0.5), outs=[C.neghalf])
    K.op("pool", lambda e: e.affine_select(out=C.ident[:], in_=C.ones_f[:], pattern=[[-1, 128]],
                                           compare_op=ALU.is_equal, fill=0.0, base=0, channel_multiplier=1),
         outs=[C.ident], ins=[C.ones_f])
    K.op("pool", lambda e: e.tensor_scalar(out=C.nident[:], in0=C.ident[:], scalar1=-1.0, scalar2=None, op0=ALU.mult),
         outs=[C.nident], ins=[C.ident])


def phase_kv(C):
    K = C.K
    P = C.P
    with ExitStack() as st:
        wkv = sb(C, st, [128, 8, 2048], BF16, "wkv")
        load_wbf(C, wkv, P["w_mem_kv"], 2048)
        mt_ = [sb(C, st, [128, 1024], F32, "memt") for _ in range(2)]
        memT = sb(C, st, [128, 8, 256], BF16, "memT")
        for s in range(2):
            for mt in range(2):
                t = mt_[mt]
                K.dma("sp", t[:], P["mem"][s * 256 + mt * 128:s * 256 + (mt + 1) * 128, :], outs=[t])
                transpose8(C, t, memT, lambda k0: memT[:, k0:k0 + 4, mt * 128:(mt + 1) * 128], C.PS[0], C.PS[1])
            for ft in range(8):
                ps = C.PS[2 + ft % 2]
                for kt in range(8):
                    mm(C, ps[:, 0:256], wkv[:, kt, ft * 128:(ft + 1) * 128], memT[:, kt, :], kt == 0, kt == 7,
                       [ps], [wkv, memT])
                act(C, C.kmT[:, s, ft, :], ps[:, 0:256], AF.Copy, [C.kmT], [ps], scale=1.0 / 16.0)
            for mt in range(2):
                for half in range(2):
                    ps = C.PS[4 + half]
                    for kt in range(8):
                        mm(C, ps[:], memT[:, kt, mt * 128:(mt + 1) * 128],
                           wkv[:, kt, 1024 + half * 512:1024 + (half + 1) * 512], kt == 0, kt == 7, [ps], [memT, wkv])
                    cp(C, "dve", C.vmem[:, s, mt, half * 512:(half + 1) * 512], ps[:], [C.vmem], [ps])
        K.barrier()


def phase_l0_attn(C, s, dT, src):
    K = C.K
    P = C.P
    with ExitStack() as st:
        wqkv = sb(C, st, [128, 8, 1536], BF16, "wqkv")
        load_wbf(C, wqkv, P["l0_w_in"], 1536, col0=1024)
        qTa = sb(C, st, [128, 4, 2048], BF16, "qT")
        kTa = sb(C, st, [128, 4, 2048], BF16, "kT")
        vda = sb(C, st, [128, 16, 512], BF16, "vd")
        qT = [Tile(qTa.ap) for _ in range(4)]
        kT = [Tile(kTa.ap) for _ in range(4)]
        vd = [Tile(vda.ap) for _ in range(4)]
        xt = [sb(C, st, [128, 1024], F32, "xt") for _ in range(3)]
        xTb = [sb(C, st, [128, 8, 512], BF16, "xTb") for _ in range(2)]
        lv = [sb(C, st, [128, 64], F32, "lv") for _ in range(4)]
        for i, nme in enumerate(("l0_lam_q1", "l0_lam_k1", "l0_lam_q2", "l0_lam_k2")):
            load_bc(C, lv[i], P[nme])
        l1 = sb(C, st, [128, 2], F32, "l1")
        neglam = sb(C, st, [128, 1], F32, "neglam")
        gsub = sb(C, st, [128, 1], F32, "gsub")
        tt(C, "dve", lv[0][:], lv[0][:], lv[1][:], ALU.mult, [lv[0]], [lv[0], lv[1]])
        tt(C, "dve", lv[2][:], lv[2][:], lv[3][:], ALU.mult, [lv[2]], [lv[2], lv[3]])
        K.op("dve", lambda e: e.reduce_sum(out=l1[:, 0:1], in_=lv[0][:], axis=AX.X), outs=[l1], ins=[lv[0]])
        K.op("dve", lambda e: e.reduce_sum(out=l1[:, 1:2], in_=lv[2][:], axis=AX.X), outs=[l1], ins=[lv[2]])
        act(C, l1[:], l1[:], AF.Exp, [l1], [l1])
        tt(C, "dve", neglam[:], l1[:, 1:2], l1[:, 0:1], ALU.subtract, [neglam], [l1])
        ts(C, "dve", neglam[:], neglam[:], -LAM_INIT, None, ALU.add, None, [neglam], [neglam])
        K.dma("sp", gsub[:], P["l0_subln_g"].rearrange("(p o) -> p o", o=1), outs=[gsub])
        ts(C, "dve", gsub[:], gsub[:], 1.0 - LAM_INIT, None, ALU.mult, None, [gsub], [gsub])

        nx = 0
        for blk in range(4):
            xb_ = xTb[blk % 2]
            for t4 in range(4):
                x = xt[nx % 3]
                nx += 1
                r0 = s * SEQ + blk * 512 + t4 * 128
                K.dma("sp", x[:], src[r0:r0 + 128, :], outs=[x])
                transpose8(C, x, xb_, lambda k0: xb_[:, k0:k0 + 4, t4 * 128:(t4 + 1) * 128], C.PS[0], C.PS[1])
            for h in range(4):
                ps = C.PS[2 + h % 2]
                for kt in range(8):
                    mm(C, ps[:], wqkv[:, kt, h * 128:(h + 1) * 128], xb_[:, kt, :], kt == 0, kt == 7, [ps], [wqkv, xb_])
                act(C, qTa[:, h, blk * 512:(blk + 1) * 512], ps[:], AF.Copy, [qT[blk]], [ps], scale=0.125)
                ps = C.PS[4 + h % 2]
                for kt in range(8):
                    mm(C, ps[:], wqkv[:, kt, 512 + h * 128:512 + (h + 1) * 128], xb_[:, kt, :], kt == 0, kt == 7,
                       [ps], [wqkv, xb_])
                cp(C, "dve", kTa[:, h, blk * 512:(blk + 1) * 512], ps[:], [kT[blk]], [ps])
            for t4 in range(4):
                ps = C.PS[6 + t4 % 2]
                for kt in range(8):
                    mm(C, ps[:], xb_[:, kt, t4 * 128:(t4 + 1) * 128], wqkv[:, kt, 1024:1536], kt == 0, kt == 7,
                       [ps], [xb_, wqkv])
                cp(C, "act" if t4 % 2 else "dve", vda[:, blk * 4 + t4, :], ps[:], [vd[blk]], [ps])

        pT = [sb(C, st, [128, 512], BF16, "pT") for _ in range(4)]
        rc = [sb(C, st, [128, 512], F32, "rc") for _ in range(2)]
        tq = [sb(C, st, [128, 512], F32, "tq") for _ in range(2)]
        o_ = sb(C, st, [128, 512], F32, "o")
        osq = sb(C, st, [128, 512], BF16, "osq")
        v1 = sb(C, st, [128, 512], F32, "v1")
        rstd = sb(C, st, [128, 512], F32, "rstd")
        NT = [C.PS[2], C.PS[3]]
        DT = [C.PS[4], C.PS[5]]
        cnt = 0
        SB = [C.PS[0], C.PS[1], C.PS[7]]
        for h in range(4):
            for qb in range(4):
                last = 4 * qb + 3
                items = [(kt, c) for kt in range(last + 1) for c in range(2)]

                def score(kt, c, n):
                    i = kt - 4 * qb
                    q0 = max(i, 0) * 128
                    kb = kt // 4
                    sbk = SB[n % 3]
                    p = pT[n % 4]
                    mm(C, sbk[:, q0:512], kTa[c * 64:(c + 1) * 64, h, kt * 128:(kt + 1) * 128],
                       qTa[c * 64:(c + 1) * 64, h, qb * 512 + q0:(qb + 1) * 512], True, True,
                       [sbk], [kT[kb], qT[qb]])
                    act(C, p[:, q0:512], sbk[:, q0:512], AF.Exp, [p], [sbk])
                    if i >= 0:
                        K.op("pool", lambda e: e.affine_select(out=p[:, q0:q0 + 128], in_=p[:, q0:q0 + 128],
                                                               pattern=[[1, 128]], compare_op=ALU.is_ge, fill=0.0,
                                                               base=0, channel_multiplier=-1), outs=[p], ins=[p])

                def accum(kt, c, n):
                    i = kt - 4 * qb
                    q0 = max(i, 0) * 128
                    kb = kt // 4
                    p = pT[n % 4]
                    mm(C, NT[c][:, q0:512], vda[:, kt, h * 128:(h + 1) * 128], p[:, q0:512], kt == 0, kt == last,
                       [NT[c]], [vd[kb], p], inc=(kt == last))
                    mm(C, DT[c][:, q0:512], C.ones_b[:], p[:, q0:512], kt == 0, kt == last,
                       [DT[c]], [C.ones_b, p], inc=True)

                score(items[0][0], items[0][1], cnt)
                for ii, (kt, c) in enumerate(items):
                    if ii + 1 < len(items):
                        score(items[ii + 1][0], items[ii + 1][1], cnt + 1)
                    accum(kt, c, cnt)
                    cnt += 1
                for c in range(2):
                    act(C, rc[c][:], DT[c][:], AF.Ln, [rc[c]], [DT[c]])
                    act(C, rc[c][:], rc[c][:], AF.Exp, [rc[c]], [rc[c]], scale=-1.0)
                    tt(C, "dve", tq[c][:], NT[c][:], rc[c][:], ALU.mult, [tq[c]], [NT[c], rc[c]])
                stt(C, o_[:], tq[1][:], neglam[:, 0:1], tq[0][:], ALU.mult, ALU.add, [o_], [tq[0], tq[1], neglam])
                tt(C, "dve", osq[:], o_[:], o_[:], ALU.mult, [osq], [o_])
                mm(C, C.PS[6][:], C.ones_b[:], osq[:], True, True, [C.PS[6]], [C.ones_b, osq])
                ts(C, "dve", v1[:], C.PS[6][:], 1.0 / 128.0, EPS, ALU.mult, ALU.add, [v1], [C.PS[6]])
                act(C, v1[:], v1[:], AF.Ln, [v1], [v1])
                act(C, rstd[:], v1[:], AF.Exp, [rstd], [v1], scale=-0.5)
                stt(C, dT[:, h, qb * 512:(qb + 1) * 512], o_[:], gsub[:, 0:1], rstd[:], ALU.mult, ALU.mult,
                    [dT], [o_, gsub, rstd])
        K.barrier()


def phase_l0_sgu_out(C, s, dT, src, dst):
    K = C.K
    P = C.P
    with ExitStack() as st:
        wuv = sb(C, st, [128, 8, 1024], BF16, "wuv")
        load_wbf(C, wuv, P["l0_w_in"], 1024, col0=0)
        wout = sb(C, st, [128, 8, 1024], BF16, "wout")
        load_wbf(C, wout, P["l0_w_out"], 1024)
        g1 = sb(C, st, [128, 1024], F32, "g1")
        b1 = sb(C, st, [128, 1024], F32, "b1")
        load_bc(C, g1, P["l0_ln1_g"])
        load_bc(C, b1, P["l0_ln1_b"])
        sg_ = sb(C, st, [128, 512], F32, "sg")
        sbb = sb(C, st, [128, 512], F32, "sbb")
        bsp = sb(C, st, [128, 512], F32, "bsp")
        load_bc(C, sg_, P["l0_sgu_ln_g"])
        load_bc(C, sbb, P["l0_sgu_ln_b"])
        load_bc(C, bsp, P["l0_b_spatial"].rearrange("g t -> (g t)"))
        wsp = sb(C, st, [128, 4, 128], F32, "wsp")
        WTf = sb(C, st, [128, 4, 128], F32, "WTf")
        WT = sb(C, st, [128, 4, 128], BF16, "WT")
        K.dma("sp", wsp[:], P["l0_w_spatial"].rearrange("g t s -> t g s"), outs=[wsp])
        for g in range(4):
            K.op("pe", lambda e: e.transpose(out=C.PS[0][:, g * 128:(g + 1) * 128], in_=wsp[:, g, :], identity=C.ident[:]),
                 outs=[C.PS[0]], ins=[wsp, C.ident], inc=(g == 3))
        cp(C, "dve", WTf[:], C.PS[0][:].rearrange("p (g t) -> p g t", g=4), [WTf], [C.PS[0]])
        K.op("pool", lambda e: e.affine_select(out=WT[:], in_=WTf[:], pattern=[[0, 4], [1, 128]], compare_op=ALU.is_ge,
                                               fill=0.0, base=0, channel_multiplier=-1), outs=[WT], ins=[WTf])
        xt = [sb(C, st, [128, 1024], F32, "xt") for _ in range(8)]
        xTb = [sb(C, st, [128, 8, 512], BF16, "xTb") for _ in range(2)]
        ug = [sb(C, st, [128, 4, 512], BF16, "ug") for _ in range(2)]
        GTs = [GeluTmp(C, st, 512) for _ in range(2)]
        vgs = [sb(C, st, [128, 512], F32, "vg") for _ in range(2)]
        st4s = [sb(C, st, [128, 4, 6], F32, "st4") for _ in range(2)]
        mv4s = [sb(C, st, [128, 4, 2], F32, "mv4") for _ in range(2)]
        vp4s = [sb(C, st, [128, 4], F32, "vp4") for _ in range(2)]
        rs4s = [sb(C, st, [128, 4], F32, "rs4") for _ in range(2)]
        vns = [sb(C, st, [128, 512], BF16, "vn") for _ in range(2)]
        gss = [sb(C, st, [128, 512], F32, "gs") for _ in range(2)]
        aT = [sb(C, st, [128, 4, 128], BF16, "aT") for _ in range(2)]
        zs = [sb(C, st, [128, 1024], F32, "z") for _ in range(2)]
        LTs = [LNTmp(C, st) for _ in range(2)]
        ot = [sb(C, st, [128, 1024], F32, "ot") for _ in range(2)]
        no = 0
        ngl = 0

        def load_block(blk):
            xb2 = xTb[blk % 2]
            for t4 in range(4):
                x = xt[(blk % 2) * 4 + t4]
                r0 = s * SEQ + blk * 512 + t4 * 128
                K.dma("sp", x[:], src[r0:r0 + 128, :], outs=[x])
                transpose8(C, x, xb2, lambda k0: xb2[:, k0:k0 + 4, t4 * 128:(t4 + 1) * 128], C.PS[0], C.PS[1])

        load_block(0)
        for blk in range(4):
            xb_ = xTb[blk % 2]
            xs_ = [xt[(blk % 2) * 4 + t4] for t4 in range(4)]
            u = ug[blk % 2]
            for g in range(4):
                ps = C.PS[2 + g % 2]
                for kt in range(8):
                    mm(C, ps[:], wuv[:, kt, g * 128:(g + 1) * 128], xb_[:, kt, :], kt == 0, kt == 7, [ps], [wuv, xb_])
                gelu(C, ps[:], [ps], u[:, g, :], [u], GTs[ngl % 2], 512)
                ngl += 1
            if blk + 1 < 4:
                load_block(blk + 1)
            def stage1(t4):
                GT = GTs[t4 % 2]
                vg, st4, mv4, vp4, rs4 = vgs[t4 % 2], st4s[t4 % 2], mv4s[t4 % 2], vp4s[t4 % 2], rs4s[t4 % 2]
                vn = vns[t4 % 2]
                ps = C.PS[4]
                for kt in range(8):
                    mm(C, ps[:], xb_[:, kt, t4 * 128:(t4 + 1) * 128], wuv[:, kt, 512:1024], kt == 0, kt == 7,
                       [ps], [xb_, wuv])
                gelu(C, ps[:], [ps], vg[:], [vg], GT, 512)
                for g in range(4):
                    K.op("dve", lambda e: e.bn_stats(out=st4[:, g, :], in_=vg[:, g * 128:(g + 1) * 128]),
                         outs=[st4], ins=[vg])
                for g in range(4):
                    K.op("dve", lambda e: e.bn_aggr(out=mv4[:, g, :], in_=st4[:, g, :]), outs=[mv4], ins=[st4])
                ts(C, "dve", vp4[:], mv4[:, :, 1], EPS, None, ALU.add, None, [vp4], [mv4])
                act(C, rs4[:], vp4[:], AF.Ln, [rs4], [vp4])
                act(C, rs4[:], rs4[:], AF.Exp, [rs4], [rs4], scale=-0.5)
                for g in range(4):
                    gsl = slice(g * 128, (g + 1) * 128)
                    stt(C, vg[:, gsl], vg[:, gsl], mv4[:, g, 0:1], sg_[:, gsl], ALU.subtract, ALU.mult, [vg], [vg, mv4, sg_])
                    stt(C, vn[:, gsl], vg[:, gsl], rs4[:, g:g + 1], sbb[:, gsl], ALU.mult, ALU.add, [vn], [vg, rs4, sbb])

            def stage2(t4):
                nonlocal no
                x = xs_[t4]
                vn, gs, z, LT = vns[t4 % 2], gss[t4 % 2], zs[t4 % 2], LTs[t4 % 2]
                psg = C.PS[5]
                for g in range(4):
                    mm(C, psg[:, g * 128:(g + 1) * 128], vn[:, g * 128:(g + 1) * 128], WT[:, g, :], True, True,
                       [psg], [vn, WT], inc=(g == 3))
                tt(C, "dve", gs[:], psg[:], bsp[:], ALU.add, [gs], [psg, bsp])
                a = aT[t4 % 2]
                tt(C, "pool", a[:], gs[:].rearrange("p (g t) -> p g t", g=4), u[:, :, t4 * 128:(t4 + 1) * 128], ALU.mult,
                   [a], [gs, u])
                tok0 = blk * 512 + t4 * 128
                for half in range(2):
                    ps = C.PS[6 + half]
                    for ft in range(4):
                        mm(C, ps[:], a[:, ft, :], wout[:, ft, half * 512:(half + 1) * 512], ft == 0, False,
                           [ps], [a, wout], inc=False)
                    for ft in range(4):
                        mm(C, ps[:], dT[:, ft, tok0:tok0 + 128], wout[:, 4 + ft, half * 512:(half + 1) * 512], False,
                           ft == 3, [ps], [dT, wout], inc=(ft == 3))
                    stt(C, z[:, half * 512:(half + 1) * 512], x[:, half * 512:(half + 1) * 512], ALPHA, ps[:],
                        ALU.mult, ALU.add, [z], [x, ps])
                o = ot[no % 2]
                no += 1
                layernorm(C, z, o, LT, g1, b1)
                r0 = s * SEQ + tok0
                K.dma("sp", dst[r0:r0 + 128, :], o[:], ins=[o])

            stage1(0)
            for t4 in range(4):
                if t4 + 1 < 4:
                    stage1(t4 + 1)
                stage2(t4)
        K.barrier()


def phase_cross(C, L, src, dst):
    K = C.K
    P = C.P
    with ExitStack() as st:
        wxq = sb(C, st, [128, 8, 1024], BF16, "wxq")
        wxo = sb(C, st, [128, 8, 1024], BF16, "wxo")
        load_wbf(C, wxq, P[L + "xq"], 1024)
        load_wbf(C, wxo, P[L + "xo"], 1024)
        g2 = sb(C, st, [128, 1024], F32, "g2")
        b2 = sb(C, st, [128, 1024], F32, "b2")
        load_bc(C, g2, P[L + "ln2_g"])
        load_bc(C, b2, P[L + "ln2_b"])
        xt = [sb(C, st, [128, 1024], F32, "xt") for _ in range(8)]
        xTb = [sb(C, st, [128, 8, 512], BF16, "xTb") for _ in range(2)]
        qT = [sb(C, st, [128, 8, 512], BF16, "qT") for _ in range(2)]
        pT = [sb(C, st, [128, 2, 512], BF16, "pT") for _ in range(2)]
        oT = [sb(C, st, [128, 8, 512], BF16, "oT") for _ in range(2)]
        rcs = [sb(C, st, [128, 512], F32, "rc") for _ in range(2)]
        zs = [sb(C, st, [128, 1024], F32, "z") for _ in range(2)]
        LTs = [LNTmp(C, st) for _ in range(2)]
        ot = [sb(C, st, [128, 1024], F32, "ot") for _ in range(2)]
        no = [0]

        def stageA(blk, after_head=None):
            s = blk // 4
            xb_ = xTb[blk % 2]
            for t4 in range(4):
                x = xt[(blk % 2) * 4 + t4]
                r0 = blk * 512 + t4 * 128
                K.dma("sp", x[:], src[r0:r0 + 128, :], outs=[x])
                transpose8(C, x, xb_, lambda k0: xb_[:, k0:k0 + 4, t4 * 128:(t4 + 1) * 128], C.PS[0], C.PS[1])
            q = qT[blk % 2]
            for ft in range(8):
                ps = C.PS[2 + ft % 2]
                for kt in range(8):
                    mm(C, ps[:], wxq[:, kt, ft * 128:(ft + 1) * 128], xb_[:, kt, :], kt == 0, kt == 7, [ps], [wxq, xb_])
                cp(C, "act" if ft % 2 else "dve", q[:, ft, :], ps[:], [q], [ps])
            o_ = oT[blk % 2]

            def sc(h):
                p = pT[h % 2]
                for mt in range(2):
                    ps = C.PS[4 + mt]
                    for j in range(2):
                        mm(C, ps[:], C.kmT[:, s, h * 2 + j, mt * 128:(mt + 1) * 128], q[:, h * 2 + j, :], j == 0, j == 1,
                           [ps], [C.kmT, q])
                    act(C, p[:, mt, :], ps[:], AF.Exp, [p], [ps])

            def pv(h):
                p = pT[h % 2]
                rc = rcs[h % 2]
                psd = C.PS[6]
                for mt in range(2):
                    mm(C, psd[:], C.ones_b[:], p[:, mt, :], mt == 0, mt == 1, [psd], [C.ones_b, p])
                act(C, rc[:], psd[:], AF.Ln, [rc], [psd])
                act(C, rc[:], rc[:], AF.Exp, [rc], [rc], scale=-1.0)
                for j in range(2):
                    ps = C.PS[(7, 0)[j]]
                    for mt in range(2):
                        mm(C, ps[:], C.vmem[:, s, mt, h * 256 + j * 128:h * 256 + (j + 1) * 128], p[:, mt, :], mt == 0,
                           mt == 1, [ps], [C.vmem, p])
                    tt(C, "dve", o_[:, h * 2 + j, :], ps[:], rc[:], ALU.mult, [o_], [ps, rc])

            sc(0)
            for h in range(4):
                if h + 1 < 4:
                    sc(h + 1)
                pv(h)
                if after_head is not None:
                    after_head(h)

        def stageB(blk, only=None):
            o_ = oT[blk % 2]
            for t4 in (range(4) if only is None else [only]):
                x = xt[(blk % 2) * 4 + t4]
                z = zs[t4 % 2]
                LT = LTs[t4 % 2]
                for half in range(2):
                    ps = C.PS[2 + half]
                    for ft in range(8):
                        mm(C, ps[:], o_[:, ft, t4 * 128:(t4 + 1) * 128], wxo[:, ft, half * 512:(half + 1) * 512], ft == 0,
                           ft == 7, [ps], [o_, wxo])
                    stt(C, z[:, half * 512:(half + 1) * 512], x[:, half * 512:(half + 1) * 512], ALPHA, ps[:],
                        ALU.mult, ALU.add, [z], [x, ps])
                o = ot[no[0] % 2]
                no[0] += 1
                layernorm(C, z, o, LT, g2, b2, rs_eng="act")
                r0 = blk * 512 + t4 * 128
                K.dma("sp", dst[r0:r0 + 128, :], o[:], ins=[o])

        stageA(0)
        for blk in range(8):
            if blk + 1 < 8:
                stageA(blk + 1, after_head=lambda h, b=blk: stageB(b, only=h))
            else:
                stageB(blk)
        K.barrier()


NRING = 10


def phase_moe(C, L, src, dst):
    K = C.K
    P = C.P
    with ExitStack() as st:
        g3 = sb(C, st, [128, 1024], F32, "g3")
        b3 = sb(C, st, [128, 1024], F32, "b3")
        load_bc(C, g3, P[L + "ln3_g"])
        load_bc(C, b3, P[L + "ln3_b"])
        rw = sb(C, st, [128, 8, 32], F32, "rw")
        K.dma("sp", rw[:], P[L + "router_w"].rearrange("(kt p) e -> p kt e", p=128), outs=[rw])
        rb = sb(C, st, [128, 32], F32, "rb")
        load_bc(C, rb, P[L + "router_b"])
        bd = sb(C, st, [32, 1024], BF16, "bd")
        bupT = sb(C, st, [128, 16, 32], F32, "bupT")
        with ExitStack() as st2:
            bdf = sb(C, st2, [32, 1024], F32, "bdf")
            K.dma("sp", bdf[:], P[L + "exp_b_down"], outs=[bdf])
            cp(C, "dve", bd[:], bdf[:], [bd], [bdf])
            buf_ = sb(C, st2, [32, 2048], F32, "buf")
            K.dma("sp", buf_[:], P[L + "exp_b_up"], outs=[buf_])
            for ft in range(16):
                ps = C.PS[ft % 2]
                K.op("pe", lambda e: e.transpose(out=ps[:, 0:32], in_=buf_[:, ft * 128:(ft + 1) * 128],
                                                 identity=C.ident[0:32, 0:32]), outs=[ps], ins=[buf_, C.ident])
                cp(C, "dve", bupT[:, ft, :], ps[:, 0:32], [bupT], [ps])
            K.barrier()

        ringU = [sb(C, st, [128, 4096], BF16, "ringU") for _ in range(8)]
        ringD = [sb(C, st, [128, 4096], BF16, "ringD") for _ in range(2)]
        xt = [sb(C, st, [128, 1024], F32, "xt") for _ in range(2)]
        x2T = [sb(C, st, [128, 8, 512], BF16, "x2T") for _ in range(1)]
        x2Tf = sb(C, st, [128, 8, 128], F32, "x2Tf")
        yacc = [sb(C, st, [128, 1024], F32, "yacc") for _ in range(4)]
        gates = sb(C, st, [128, 4, 32], F32, "gates")
        gT = sb(C, st, [32, 4, 128], BF16, "gT")
        lg = sb(C, st, [128, 32], F32, "lg")
        mx8 = sb(C, st, [128, 8], F32, "mx8")
        msk = sb(C, st, [128, 32], F32, "msk")
        nmx = sb(C, st, [128, 1], F32, "nmx")
        ssum = sb(C, st, [128, 1], F32, "ssum")
        actT = sb(C, st, [128, 8, 512], BF16, "actT")
        actTt = [Tile(actT.ap) for _ in range(8)]
        ev = [dict(g=sb(C, st, [128, 512], F32, "evg"), s=sb(C, st, [128, 512], F32, "evs"),
                   l=sb(C, st, [128, 512], F32, "evl"), t=sb(C, st, [128, 512], F32, "evt")) for _ in range(2)]
        LT = LNTmp(C, st)
        ot = [sb(C, st, [128, 1024], F32, "ot") for _ in range(2)]
        wup = P[L + "exp_w_up"]
        wdn = P[L + "exp_w_down"]
        nring = [0]

        def load_up(e):
            ups = []
            for c in (0, 2, 1, 3):
                t = ringU[nring[0] % 8]
                nring[0] += 1
                K.dma("pool", t[:].rearrange("p (kt n) -> p kt n", kt=8),
                      wup[e, :, c * 512:(c + 1) * 512].rearrange("(kt p) n -> p kt n", p=128), outs=[t])
                ups.append((c, t))
            ups = dict(ups)
            return [ups[c] for c in range(4)]

        def load_dn(e):
            dns = []
            for c in range(2):
                t = ringD[c]
                K.dma("pool", t[:].rearrange("p (ft n) -> p ft n", ft=4),
                      wdn[e, c * 512:(c + 1) * 512, :].rearrange("(ft p) n -> p ft n", p=128), outs=[t])
                dns.append(t)
            return dns

        no = 0
        nev = 0
        for blk in range(8):
            xT_ = x2T[0]
            for t4 in range(4):
                x = xt[t4 % 2]
                r0 = blk * 512 + t4 * 128
                K.dma("sp", x[:], src[r0:r0 + 128, :], outs=[x])

                def extra(k0, bank):
                    cp(C, "act" if k0 else "dve", x2Tf[:, k0:k0 + 4, :], bank[:].rearrange("p (k n) -> p k n", k=4),
                       [x2Tf], [bank])
                transpose8(C, x, xT_, lambda k0: xT_[:, k0:k0 + 4, t4 * 128:(t4 + 1) * 128], C.PS[6], C.PS[7],
                           extra=extra)
                act(C, yacc[t4][:], x[:], AF.Copy, [yacc[t4]], [x], scale=ALPHA)
                ps = C.PS[6]
                for kt in range(8):
                    mm(C, ps[:, 0:32], x2Tf[:, kt, :], rw[:, kt, :], kt == 0, kt == 7, [ps], [x2Tf, rw])
                tt(C, "dve", lg[:], ps[:, 0:32], rb[:], ALU.add, [lg], [ps, rb])
                K.op("dve", lambda e: e.max(out=mx8[:], in_=lg[:]), outs=[mx8], ins=[lg])
                ts(C, "dve", msk[:], lg[:], mx8[:, 3:4], None, ALU.is_ge, None, [msk], [lg, mx8])
                ts(C, "dve", nmx[:], mx8[:, 0:1], -1.0, None, ALU.mult, None, [nmx], [mx8])
                act(C, lg[:], lg[:], AF.Exp, [lg], [lg, nmx], bias=nmx[:, 0:1])
                tt(C, "dve", lg[:], lg[:], msk[:], ALU.mult, [lg], [lg, msk])
                K.op("dve", lambda e: e.reduce_sum(out=ssum[:], in_=lg[:], axis=AX.X), outs=[ssum], ins=[lg])
                K.op("dve", lambda e: e.reciprocal(ssum[:], ssum[:]), outs=[ssum], ins=[ssum])
                ts(C, "dve", gates[:, t4, :], lg[:], ssum[:, 0:1], None, ALU.mult, None, [gates], [lg, ssum])
                ps = C.PS[7]
                K.op("pe", lambda e: e.transpose(out=ps[0:32, 0:128], in_=gates[:, t4, :], identity=C.ident[:]),
                     outs=[ps], ins=[gates, C.ident])
                cp(C, "dve", gT[:, t4, :], ps[0:32, 0:128], [gT], [ps])
                for half in range(2):
                    ps = C.PS[6 + half]
                    mm(C, ps[:], gT[:, t4, :], bd[:, half * 512:(half + 1) * 512], True, True, [ps], [gT, bd])
                    tt(C, "dve", yacc[t4][:, half * 512:(half + 1) * 512], yacc[t4][:, half * 512:(half + 1) * 512],
                       ps[:], ALU.add, [yacc[t4]], [yacc[t4], ps])
            nxt_u = load_up(0)
            dns = load_dn(0)
            for e_ in range(32):
                ups = nxt_u
                if e_ + 1 < 32:
                    nxt_u = load_up(e_ + 1)
                for j in range(8):
                    E = ev[nev % 2]
                    nev += 1
                    pg = C.PS[(nev % 2) * 2]
                    pl = C.PS[(nev % 2) * 2 + 1]
                    wg = ups[j // 4]
                    wl = ups[2 + j // 4]
                    c0 = (j % 4) * 128
                    for kt in range(8):
                        mm(C, pg[:], wg[:, kt * 512 + c0:kt * 512 + c0 + 128], xT_[:, kt, :], kt == 0, kt == 7,
                           [pg], [wg, xT_])
                    for kt in range(8):
                        mm(C, pl[:], wl[:, kt * 512 + c0:kt * 512 + c0 + 128], xT_[:, kt, :], kt == 0, kt == 7,
                           [pl], [wl, xT_])
                    ts(C, "dve", E["g"][:], pg[:], bupT[:, j, e_:e_ + 1], 7.0, ALU.add, ALU.min, [E["g"]], [pg, bupT])
                    act(C, E["s"][:], E["g"][:], AF.Sigmoid, [E["s"]], [E["g"]], scale=1.702)
                    act(C, E["l"][:], pl[:], AF.Identity, [E["l"]], [pl, bupT], bias=bupT[:, 8 + j, e_:e_ + 1])
                    ts(C, "dve", E["l"][:], E["l"][:], 7.0, -7.0, ALU.min, ALU.max, [E["l"]], [E["l"]])
                    tt(C, "dve", E["t"][:], E["g"][:], E["s"][:], ALU.mult, [E["t"]], [E["g"], E["s"]])
                    stt(C, actT[:, j, :], E["l"][:], 1.0, E["t"][:], ALU.add, ALU.mult, [actTt[j]], [E["l"], E["t"]])
                for t4 in range(4):
                    for half in range(2):
                        ps = C.PS[4 + half]
                        for ft in range(8):
                            wd = dns[ft // 4]
                            f0 = (ft % 4) * 1024 + half * 512
                            mm(C, ps[:], actT[:, ft, t4 * 128:(t4 + 1) * 128], wd[:, f0:f0 + 512], ft == 0, ft == 7,
                               [ps], [actTt[ft], wd])
                        stt(C, yacc[t4][:, half * 512:(half + 1) * 512], ps[:], gates[:, t4, e_:e_ + 1],
                            yacc[t4][:, half * 512:(half + 1) * 512], ALU.mult, ALU.add, [yacc[t4]],
                            [ps, gates, yacc[t4]])
                if e_ + 1 < 32:
                    dns = load_dn(e_ + 1)
            for t4 in range(4):
                o = ot[no % 2]
                no += 1
                layernorm(C, yacc[t4], o, LT, g3, b3)
                r0 = blk * 512 + t4 * 128
                K.dma("sp", dst[r0:r0 + 128, :], o[:], ins=[o])
        K.barrier()


U32 = mybir.dt.uint32
NBLK = 64
BSZ = 512


def phase_moe_sparse(C, L, src, dst, Xs, Ys):
    K = C.K
    P = C.P
    IOA = bass.IndirectOffsetOnAxis
    wup2 = P[L + "exp_w_up"].rearrange("e k n -> (e k) n")
    wdn2 = P[L + "exp_w_down"].rearrange("e k n -> (e k) n")
    with ExitStack() as stp:
        idx_all = sb(C, stp, [128, 32, 4], I32, "idxall")
        gsel_all = sb(C, stp, [128, 32, 4], F32, "gselall")
        idw = sb(C, stp, [128, NBLK, 8], I32, "idw")
        oh = sb(C, stp, [32, NBLK], F32, "oh")
        bupb = sb(C, stp, [32, 2048], BF16, "bupb")
        bdnb = sb(C, stp, [32, 1024], BF16, "bdnb")
        identb = sb(C, stp, [128, 128], BF16, "identb")
        cp(C, "dve", identb[:], C.ident[:], [identb], [C.ident])
        gT_all = sb(C, stp, [32, 32, 128], BF16, "gTall")
        ohb16 = sb(C, stp, [32, NBLK], BF16, "ohb16")
        ringU = [sb(C, stp, [128, 8, 2048], BF16, "ringU") for _ in range(2)]
        ringD = [sb(C, stp, [128, 8, 1024], BF16, "ringD") for _ in range(2)]
        ringUt = [[Tile(r.ap) for _ in range(8)] for r in ringU]
        ringDt = [[Tile(r.ap) for _ in range(8)] for r in ringD]
        bc_reg = C.nc.gpsimd.to_reg(32 * 1024 - 1)

        def load_w(b):
            wu = ringU[b % 2]
            wd = ringD[b % 2]
            for kt in range(8):
                K.dmaf("pool", lambda e: e.indirect_dma_start(out=wu[:, kt, :], out_offset=None, in_=wup2[:, :],
                                                              in_offset=IOA(ap=idw[:, b, kt:kt + 1].bitcast(U32), axis=0),
                                                              bounds_check=bc_reg, oob_is_err=False),
                       outs=[ringUt[b % 2][kt]], ins=[idw])
            for kt in range(8):
                K.dmaf("pool", lambda e: e.indirect_dma_start(out=wd[:, kt, :], out_offset=None, in_=wdn2[:, :],
                                                              in_offset=IOA(ap=idw[:, b, kt:kt + 1].bitcast(U32), axis=0),
                                                              bounds_check=bc_reg, oob_is_err=False),
                       outs=[ringDt[b % 2][kt]], ins=[idw])

        with ExitStack() as st:
            rw = sb(C, st, [128, 8, 32], F32, "rw")
            K.dma("sp", rw[:], P[L + "router_w"].rearrange("(kt p) e -> p kt e", p=128), outs=[rw])
            rb = sb(C, st, [128, 32], F32, "rb")
            load_bc(C, rb, P[L + "router_b"])
            bf_ = sb(C, st, [32, 2048], F32, "bf_")
            K.dma("sp", bf_[:], P[L + "exp_b_up"], outs=[bf_])
            cp(C, "dve", bupb[:], bf_[:], [bupb], [bf_])
            K.dma("sp", bf_[:, 0:1024], P[L + "exp_b_down"], outs=[bf_])
            cp(C, "dve", bdnb[:], bf_[:, 0:1024], [bdnb], [bf_])
            Ust = sb(C, st, [128, 128], BF16, "Ust")
            K.op("pool", lambda e: e.affine_select(out=Ust[:], in_=C.ones_b[:], pattern=[[1, 128]], compare_op=ALU.is_ge,
                                                   fill=0.0, base=-1, channel_multiplier=-1), outs=[Ust], ins=[C.ones_b])
            pid = sb(C, st, [128, 1], F32, "pid")
            K.op("pool", lambda e: e.iota(pid[:], pattern=[[0, 1]], base=0, channel_multiplier=1,
                                          allow_small_or_imprecise_dtypes=True), outs=[pid])
            base = sb(C, st, [128, 32], F32, "base")
            K.op("dve", lambda e: e.memset(base[:], 0.0), outs=[base])
            gates_all = sb(C, st, [128, 32, 32], F32, "gatesall")
            msk_all = sb(C, st, [128, 32, 32], F32, "mskall")
            rank_all = sb(C, st, [128, 32, 32], F32, "rankall")
            xt = [sb(C, st, [128, 1024], F32, "xt") for _ in range(4)]
            xb16 = [sb(C, st, [128, 1024], BF16, "xb16") for _ in range(2)]

            def ldx(t):
                if t < 32:
                    K.dma("sp", xt[t % 4][:], src[t * 128:(t + 1) * 128, :], outs=[xt[t % 4]])
            x2Tfs = [sb(C, st, [128, 8, 128], F32, "x2Tf") for _ in range(2)]
            lgs = [sb(C, st, [128, 32], F32, "lg") for _ in range(2)]
            mx8s = [sb(C, st, [128, 8], F32, "mx8") for _ in range(2)]
            nmxs = [sb(C, st, [128, 1], F32, "nmx") for _ in range(2)]
            ssums = [sb(C, st, [128, 1], F32, "ssum") for _ in range(2)]
            mskbs = [sb(C, st, [128, 32], BF16, "mskb") for _ in range(2)]
            def rstage1(t):
                x = xt[t % 4]
                x2Tf = x2Tfs[t % 2]
                transpose8(C, x, x2Tf, lambda k0: x2Tf[:, k0:k0 + 4, :], C.PS[0], C.PS[1])
                ps = C.PS[2 + t % 2]
                for kt in range(8):
                    mm(C, ps[:, 0:32], x2Tf[:, kt, :], rw[:, kt, :], kt == 0, kt == 7, [ps], [x2Tf, rw])

            ldx(0)
            ldx(1)
            rstage1(0)
            for t in range(32):
                ldx(t + 2)
                if t + 1 < 32:
                    rstage1(t + 1)
                x2Tf, lg, mx8, nmx, ssum, mskb = x2Tfs[t % 2], lgs[t % 2], mx8s[t % 2], nmxs[t % 2], ssums[t % 2], mskbs[t % 2]
                ps = C.PS[2 + t % 2]
                tt(C, "dve", lg[:], ps[:, 0:32], rb[:], ALU.add, [lg], [ps, rb])
                K.op("dve", lambda e: e.max(out=mx8[:], in_=lg[:]), outs=[mx8], ins=[lg])
                ts(C, "dve", msk_all[:, t, :], lg[:], mx8[:, 3:4], None, ALU.is_ge, None, [msk_all], [lg, mx8])
                ts(C, "dve", nmx[:], mx8[:, 0:1], -1.0, None, ALU.mult, None, [nmx], [mx8])
                act(C, lg[:], lg[:], AF.Exp, [lg], [lg, nmx], bias=nmx[:, 0:1])
                tt(C, "dve", lg[:], lg[:], msk_all[:, t, :], ALU.mult, [lg], [lg, msk_all])
                K.op("dve", lambda e: e.reduce_sum(out=ssum[:], in_=lg[:], axis=AX.X), outs=[ssum], ins=[lg])
                K.op("dve", lambda e: e.reciprocal(ssum[:], ssum[:]), outs=[ssum], ins=[ssum])
                ts(C, "dve", gates_all[:, t, :], lg[:], ssum[:, 0:1], None, ALU.mult, None, [gates_all], [lg, ssum])
                psg_ = C.PS[6 + t % 2]
                K.op("pe", lambda e: e.transpose(out=psg_[0:32, 0:128], in_=gates_all[:, t, :], identity=C.ident[:]),
                     outs=[psg_], ins=[gates_all, C.ident])
                cp(C, "act", gT_all[:, t, :], psg_[0:32, 0:128], [gT_all], [psg_])
                cp(C, "dve", mskb[:], msk_all[:, t, :], [mskb], [msk_all])
                ps2 = C.PS[4 + t % 2]
                mm(C, ps2[:, 0:32], Ust[:], mskb[:], True, True, [ps2], [Ust, mskb], inc=False)
                mm(C, ps2[:, 32:64], C.ones_b[:], mskb[:], True, True, [ps2], [C.ones_b, mskb], inc=True)
                tt(C, "dve", rank_all[:, t, :], ps2[:, 0:32], base[:], ALU.add, [rank_all], [ps2, base])
                tt(C, "dve", base[:], base[:], ps2[:, 32:64], ALU.add, [base], [base, ps2])
            pad = sb(C, st, [128, 32], F32, "pad")
            padi = sb(C, st, [128, 32], I32, "padi")
            pend = sb(C, st, [128, 32], F32, "pend")
            pstart = sb(C, st, [128, 32], F32, "pstart")
            ts(C, "dve", pad[:], base[:], float(BSZ - 1), 1.0 / BSZ, ALU.add, ALU.mult, [pad], [base])
            ts(C, "dve", pad[:], pad[:], -0.49951171875, None, ALU.add, None, [pad], [pad])
            cp(C, "dve", padi[:], pad[:], [padi], [pad])
            cp(C, "dve", pad[:], padi[:], [pad], [padi])
            ts(C, "dve", pad[:], pad[:], float(BSZ), None, ALU.mult, None, [pad], [pad])
            K.op("dve", lambda e: e.tensor_tensor_scan(out=pend[:], data0=C.ones_f[:, 0:32], data1=pad[:], initial=0.0,
                                                       op0=ALU.mult, op1=ALU.add), outs=[pend], ins=[C.ones_f, pad])
            tt(C, "dve", pstart[:], pend[:], pad[:], ALU.subtract, [pstart], [pend, pad])
            cmp_ = sb(C, st, [128, NBLK, 32], F32, "cmp")
            be = sb(C, st, [128, NBLK], F32, "be")
            for b in range(NBLK):
                ts(C, "dve", cmp_[:, b, :], pend[:], float(BSZ * b), None, ALU.is_le, None, [cmp_], [pend])
            K.op("dve", lambda e: e.reduce_sum(out=be[:], in_=cmp_[:], axis=AX.X), outs=[be], ins=[cmp_])
            idf = sb(C, st, [128, NBLK, 8], F32, "idf")
            for kt in range(8):
                ts(C, "dve", idf[:, :, kt], be[:], 1024.0, float(kt * 128), ALU.mult, ALU.add, [idf], [be])
            ts(C, "dve", be[:], be[:], 31.0, None, ALU.min, None, [be], [be])
            for kt in range(8):
                ts(C, "dve", idf[:, :, kt], idf[:, :, kt], pid[:, 0:1], None, ALU.add, None, [idf], [idf, pid])
            cp(C, "dve", idw[:], idf[:], [idw], [idf])
            load_w(0)
            ts(C, "dve", oh[:], be[0:32, :], pid[0:32, 0:1], None, ALU.is_equal, None, [oh], [be, pid])
            cp(C, "dve", ohb16[:], oh[:], [ohb16], [oh])
            keys = [sb(C, st, [128, 32], F32, "key") for _ in range(2)]
            junks = [sb(C, st, [128, 32], F32, "junk") for _ in range(2)]
            d4s = [sb(C, st, [128, 4], F32, "d4") for _ in range(2)]
            ldx(0)
            ldx(1)
            for t in range(32):
                ldx(t + 2)
                x = xt[t % 4]
                xb = xb16[t % 2]
                key, junk, d4, mx8 = keys[t % 2], junks[t % 2], d4s[t % 2], mx8s[t % 2]
                act(C, xb[:], x[:], AF.Copy, [xb], [x])
                tt(C, "dve", key[:], rank_all[:, t, :], pstart[:], ALU.add, [key], [rank_all, pstart])
                stt(C, key[:], key[:], 1.0, msk_all[:, t, :], ALU.add, ALU.mult, [key], [key, msk_all])
                K.op("dve", lambda e: e.max(out=mx8[:], in_=key[:]), outs=[mx8], ins=[key])
                ts(C, "dve", d4[:], mx8[:, 0:4], -1.0, None, ALU.add, None, [d4], [mx8])
                cp(C, "dve", idx_all[:, t, :], d4[:], [idx_all], [d4])
                for k in range(4):
                    K.op("dve", lambda e: e.scalar_tensor_tensor(out=junk[:], in0=key[:], scalar=mx8[:, k:k + 1],
                                                                 in1=gates_all[:, t, :], op0=ALU.is_equal, op1=ALU.mult,
                                                                 accum_out=gsel_all[:, t, k:k + 1]),
                         outs=[junk, gsel_all], ins=[key, mx8, gates_all])
                for k in range(4):
                    K.dmaf("pool", lambda e: e.indirect_dma_start(out=Xs[:, :], out_offset=IOA(ap=idx_all[:, t, k:k + 1].bitcast(U32), axis=0),
                                                                  in_=xb[:, :], in_offset=None), ins=[xb, idx_all])
            K.barrier()
        with ExitStack() as st:
            xs_t = [sb(C, st, [128, 1024], BF16, "xs") for _ in range(8)]
            xT = [sb(C, st, [128, 8, 512], BF16, "xT") for _ in range(2)]
            actT = sb(C, st, [128, 8, 512], BF16, "actT")
            actTt = [Tile(actT.ap) for _ in range(8)]
            ev = [dict(g=sb(C, st, [128, 512], F32, "evg"), s=sb(C, st, [128, 512], F32, "evs"),
                       l=sb(C, st, [128, 512], F32, "evl"), t=sb(C, st, [128, 512], F32, "evt")) for _ in range(2)]
            yout = [sb(C, st, [128, 1024], BF16, "yout") for _ in range(3)]
            bsel = [sb(C, st, [128, 16], F32, "bsel") for _ in range(2)]

            def make_bsel(b):
                bank = C.PS[4 + b % 2]
                for ft in range(16):
                    mm(C, bank[:, ft:ft + 1], bupb[:, ft * 128:(ft + 1) * 128], ohb16[:, b:b + 1], True, True,
                       [bank], [bupb, ohb16], inc=(ft == 15))
                cp(C, "dve", bsel[b % 2][:], bank[:, 0:16], [bsel[b % 2]], [bank])

            def load_x(b):
                for i in range(4):
                    xs = xs_t[(b % 2) * 4 + i]
                    r0 = b * BSZ + i * 128
                    K.dma("sp", xs[:], Xs[r0:r0 + 128, :], outs=[xs])

            def transpose_x(b):
                xT_ = xT[b % 2]
                for i in range(4):
                    xs = xs_t[(b % 2) * 4 + i]
                    bank = C.PS[6 + i % 2]
                    psb = bank[:].bitcast(BF16)
                    for kt in range(8):
                        K.op("pe", lambda e: e.transpose(out=psb[:, kt * 128:(kt + 1) * 128], in_=xs[:, kt * 128:(kt + 1) * 128],
                                                         identity=identb[:]), outs=[bank], ins=[xs, identb], inc=(kt == 7))
                    cp(C, "act" if i % 2 else "dve", xT_[:, :, i * 128:(i + 1) * 128],
                       psb.rearrange("p (k n) -> p k n", k=8), [xT_], [bank])

            load_x(0)
            transpose_x(0)
            nev = 0
            nd = 0
            ny = 0
            for b in range(NBLK):
                if b + 1 < NBLK:
                    load_w(b + 1)
                    load_x(b + 1)
                wu = ringU[b % 2]
                wd = ringD[b % 2]
                xT_ = xT[b % 2]
                make_bsel(b)
                bs_ = bsel[b % 2]
                for j in range(8):
                    E = ev[nev % 2]
                    pg = C.PS[(nev % 2) * 2]
                    pl = C.PS[(nev % 2) * 2 + 1]
                    nev += 1
                    for kt in range(8):
                        mm(C, pg[:], wu[:, kt, j * 128:(j + 1) * 128], xT_[:, kt, :], kt == 0, kt == 7, [pg],
                           [ringUt[b % 2][kt], xT_], inc=(kt == 7))
                    for kt in range(8):
                        mm(C, pl[:], wu[:, kt, 1024 + j * 128:1024 + (j + 1) * 128], xT_[:, kt, :], kt == 0, kt == 7,
                           [pl], [ringUt[b % 2][kt], xT_], inc=(kt == 7))
                    ts(C, "dve", E["g"][:], pg[:], bs_[:, j:j + 1], 7.0, ALU.add, ALU.min, [E["g"]], [pg, bs_])
                    act(C, E["s"][:], E["g"][:], AF.Sigmoid, [E["s"]], [E["g"]], scale=1.702)
                    act(C, E["l"][:], pl[:], AF.Identity, [E["l"]], [pl, bs_], bias=bs_[:, 8 + j:9 + j])
                    ts(C, "dve", E["l"][:], E["l"][:], 7.0, -7.0, ALU.min, ALU.max, [E["l"]], [E["l"]])
                    tt(C, "dve", E["t"][:], E["g"][:], E["s"][:], ALU.mult, [E["t"]], [E["g"], E["s"]])
                    stt(C, actT[:, j, :], E["l"][:], 1.0, E["t"][:], ALU.add, ALU.mult, [actTt[j]], [E["l"], E["t"]])
                if b + 1 < NBLK:
                    transpose_x(b + 1)
                for i in range(4):
                    yo = yout[ny % 3]
                    ny += 1
                    for half in range(2):
                        ps = C.PS[4 + nd % 2]
                        nd += 1
                        for ft in range(8):
                            mm(C, ps[:], actT[:, ft, i * 128:(i + 1) * 128], wd[:, ft, half * 512:(half + 1) * 512], ft == 0,
                               ft == 7, [ps], [actTt[ft], ringDt[b % 2][ft]], inc=(ft == 7))
                        act(C, yo[:, half * 512:(half + 1) * 512], ps[:], AF.Copy, [yo], [ps])
                    r0 = b * BSZ + i * 128
                    K.dma("sp", Ys[r0:r0 + 128, :], yo[:], ins=[yo])
            K.barrier()
        with ExitStack() as st:
            g3 = sb(C, st, [128, 1024], F32, "g3")
            b3 = sb(C, st, [128, 1024], F32, "b3")
            load_bc(C, g3, P[L + "ln3_g"])
            load_bc(C, b3, P[L + "ln3_b"])
            xt = [sb(C, st, [128, 1024], F32, "xt") for _ in range(4)]
            yk = [sb(C, st, [128, 1024], BF16, "yk") for _ in range(12)]

            def ldx3(t):
                if t < 32:
                    K.dma("sp", xt[t % 4][:], src[t * 128:(t + 1) * 128, :], outs=[xt[t % 4]])
            yacc = [sb(C, st, [128, 1024], F32, "yacc") for _ in range(2)]
            LTs = [LNTmp(C, st) for _ in range(2)]
            ot = [sb(C, st, [128, 1024], F32, "ot") for _ in range(2)]

            def gather(t):
                for k in range(4):
                    y_ = yk[(t % 3) * 4 + k]
                    K.dmaf("pool", lambda e: e.indirect_dma_start(out=y_[:, :], out_offset=None, in_=Ys[:, :],
                                                                  in_offset=IOA(ap=idx_all[:, t, k:k + 1].bitcast(U32), axis=0)),
                           outs=[y_], ins=[idx_all])

            def cstage1(t):
                x = xt[t % 4]
                ya = yacc[t % 2]
                act(C, ya[:], x[:], AF.Copy, [ya], [x], scale=ALPHA)
                for half in range(2):
                    psb_ = C.PS[(t % 2) * 2 + half]
                    mm(C, psb_[:], gT_all[:, t, :], bdnb[:, half * 512:(half + 1) * 512], True, True, [psb_], [gT_all, bdnb])
                    tt(C, "dve", ya[:, half * 512:(half + 1) * 512], ya[:, half * 512:(half + 1) * 512], psb_[:], ALU.add,
                       [ya], [ya, psb_])
                for k in range(4):
                    y_ = yk[(t % 3) * 4 + k]
                    stt(C, ya[:], y_[:], gsel_all[:, t, k:k + 1], ya[:], ALU.mult, ALU.add, [ya], [y_, gsel_all, ya])

            def cstage2(t):
                ya = yacc[t % 2]
                o = ot[t % 2]
                layernorm(C, ya, o, LTs[t % 2], g3, b3, rs_eng="act")
                K.dma("sp", dst[t * 128:(t + 1) * 128, :], o[:], ins=[o])

            gather(0)
            gather(1)
            ldx3(0)
            ldx3(1)
            ldx3(2)
            cstage1(0)
            for t in range(32):
                ldx3(t + 3)
                if t + 2 < 32:
                    gather(t + 2)
                if t + 1 < 32:
                    cstage1(t + 1)
                cstage2(t)
            K.barrier()


def load_T(C, st, dst_ap, dst_tile, src_view, rows):
    K = C.K
    tmp = sb(C, st, [32, 128], F32, "ltT")
    K.dma("sp", tmp[0:rows, :], src_view, outs=[tmp])
    ps = C.PS[7]
    K.op("pe", lambda e: e.transpose(out=ps[:, 0:rows], in_=tmp[0:rows, :], identity=C.ident[0:rows, 0:rows]),
         outs=[ps], ins=[tmp, C.ident])
    cp(C, "dve", dst_ap, ps[:, 0:rows], [dst_tile], [ps])


def sincos(C, th_ap, th_tiles, n, sin_ap, sin_tiles, cos_ap, cos_tiles, ta, tb, tki, thr_ap=None, thr_tiles=()):
    a, b, ki = ta[:, 0:n], tb[:, 0:n], tki[:, 0:n]
    ts(C, "dve", a, th_ap, 1.0 / (2 * PI), None, ALU.mult, None, [ta], list(th_tiles))
    cp(C, "dve", ki, a, [tki], [ta])
    cp(C, "dve", a, ki, [ta], [tki])
    stt(C, b, a, -2 * PI, th_ap, ALU.mult, ALU.add, [tb], [ta] + list(th_tiles))
    if thr_ap is not None:
        cp(C, "dve", thr_ap, b, list(thr_tiles), [tb])
    ts(C, "dve", a, b, PI_LO, -PI_LO, ALU.min, ALU.max, [ta], [tb])
    act(C, sin_ap, a, AF.Sin, list(sin_tiles), [ta])
    ts(C, "dve", a, b, PI / 2, -2 * PI, ALU.is_gt, ALU.mult, [ta], [tb])
    stt(C, a, b, PI / 2, a, ALU.add, ALU.add, [ta], [ta, tb])
    ts(C, "dve", a, a, PI_LO, -PI_LO, ALU.min, ALU.max, [ta], [ta])
    act(C, cos_ap, a, AF.Sin, list(cos_tiles), [ta])


def phase_s5(C, src, dst):
    K = C.K
    P = C.P
    with ExitStack() as st5:
        Bw_re = sb(C, st5, [128, 32, 128], BF16, "Bwre")
        Bw_im = sb(C, st5, [128, 32, 128], BF16, "Bwim")
        Cw_re = sb(C, st5, [128, 32, 128], BF16, "Cwre")
        Cw_in = sb(C, st5, [128, 32, 128], BF16, "Cwin")
        thr = sb(C, st5, [128, 32], F32, "thr")
        mag = sb(C, st5, [128, 32], F32, "mag")
        dsk = sb(C, st5, [128, 8], F32, "dsk")
        iot = sb(C, st5, [128, 513], F32, "iot")
        K.op("pool", lambda e: e.iota(iot[:], pattern=[[1, 513]], base=0, channel_multiplier=0,
                                      allow_small_or_imprecise_dtypes=True), outs=[iot])
        with ExitStack() as st:
            lr = sb(C, st, [128, 32], F32, "lr")
            li = sb(C, st, [128, 32], F32, "li")
            ldt = sb(C, st, [128, 32], F32, "ldt")
            load_T(C, st, lr[:], lr, P["l1_lambda_re"].rearrange("(m gl) p -> m (gl p)", gl=2), 32)
            load_T(C, st, li[:], li, P["l1_lambda_im"].rearrange("(m gl) p -> m (gl p)", gl=2), 32)
            load_T(C, st, dsk[:], dsk, P["l1_d_skip"].rearrange("(ct gq) c -> ct (gq c)", gq=8), 8)
            ld32 = sb(C, st, [32, 2], F32, "ld32")
            K.dma("sp", ld32[:], P["l1_log_dt"].rearrange("(m gl) -> m gl", gl=2), outs=[ld32])
            ldx = sb(C, st, [32, 128], F32, "ldx")
            for gl in range(2):
                ts(C, "dve", ldx[:, gl * 64:(gl + 1) * 64], C.ones_f[0:32, 0:64], ld32[:, gl:gl + 1], None, ALU.mult, None,
                   [ldx], [C.ones_f, ld32])
            ps = C.PS[7]
            K.op("pe", lambda e: e.transpose(out=ps[:, 0:32], in_=ldx[:, :], identity=C.ident[0:32, 0:32]),
                 outs=[ps], ins=[ldx, C.ident])
            cp(C, "dve", ldt[:], ps[:, 0:32], [ldt], [ps])
            dt = sb(C, st, [128, 32], F32, "dt")
            act(C, dt[:], ldt[:], AF.Exp, [dt], [ldt])
            tmp = sb(C, st, [128, 32], F32, "tmp")
            th = sb(C, st, [128, 32], F32, "th")
            tt(C, "dve", tmp[:], lr[:], dt[:], ALU.mult, [tmp], [lr, dt])
            act(C, mag[:], tmp[:], AF.Exp, [mag], [tmp])
            tt(C, "dve", th[:], li[:], dt[:], ALU.mult, [th], [li, dt])
            sn = sb(C, st, [128, 32], F32, "sn")
            cs = sb(C, st, [128, 32], F32, "cs")
            ta = sb(C, st, [128, 32], F32, "ta")
            tb = sb(C, st, [128, 32], F32, "tb")
            tki = sb(C, st, [128, 32], I32, "tki")
            sincos(C, th[:], [th], 32, sn[:], [sn], cs[:], [cs], ta, tb, tki, thr_ap=thr[:], thr_tiles=[thr])
            ar1 = sb(C, st, [128, 32], F32, "ar1")
            ai = sb(C, st, [128, 32], F32, "ai")
            tt(C, "dve", ar1[:], mag[:], cs[:], ALU.mult, [ar1], [mag, cs])
            ts(C, "dve", ar1[:], ar1[:], -1.0, None, ALU.add, None, [ar1], [ar1])
            tt(C, "dve", ai[:], mag[:], sn[:], ALU.mult, [ai], [mag, sn])
            den = sb(C, st, [128, 32], F32, "den")
            t2 = sb(C, st, [128, 32], F32, "t2")
            tt(C, "dve", den[:], lr[:], lr[:], ALU.mult, [den], [lr])
            tt(C, "dve", t2[:], li[:], li[:], ALU.mult, [t2], [li])
            tt(C, "dve", den[:], den[:], t2[:], ALU.add, [den], [den, t2])
            K.op("dve", lambda e: e.reciprocal(den[:], den[:]), outs=[den], ins=[den])
            zr = sb(C, st, [128, 32], F32, "zr")
            zi = sb(C, st, [128, 32], F32, "zi")
            nzi = sb(C, st, [128, 32], F32, "nzi")
            tt(C, "dve", zr[:], ar1[:], lr[:], ALU.mult, [zr], [ar1, lr])
            tt(C, "dve", t2[:], ai[:], li[:], ALU.mult, [t2], [ai, li])
            tt(C, "dve", zr[:], zr[:], t2[:], ALU.add, [zr], [zr, t2])
            tt(C, "dve", zr[:], zr[:], den[:], ALU.mult, [zr], [zr, den])
            tt(C, "dve", zi[:], ai[:], lr[:], ALU.mult, [zi], [ai, lr])
            tt(C, "dve", t2[:], ar1[:], li[:], ALU.mult, [t2], [ar1, li])
            tt(C, "dve", zi[:], zi[:], t2[:], ALU.subtract, [zi], [zi, t2])
            tt(C, "dve", zi[:], zi[:], den[:], ALU.mult, [zi], [zi, den])
            ts(C, "dve", nzi[:], zi[:], -1.0, None, ALU.mult, None, [nzi], [zi])
            Bre = sb(C, st, [128, 32, 16], F32, "Bre")
            Bim = sb(C, st, [128, 32, 16], F32, "Bim")
            K.dma("sp", Bre[:], P["l1_b_re"].rearrange("(m gl) p c -> (gl p) m c", gl=2), outs=[Bre])
            K.dma("sp", Bim[:], P["l1_b_im"].rearrange("(m gl) p c -> (gl p) m c", gl=2), outs=[Bim])
            bbr = sb(C, st, [128, 32, 16], F32, "bbr")
            bbi = sb(C, st, [128, 32, 16], F32, "bbi")
            for m in range(32):
                ts(C, "dve", bbr[:, m, :], Bre[:, m, :], zr[:, m:m + 1], None, ALU.mult, None, [bbr], [Bre, zr])
                stt(C, bbr[:, m, :], Bim[:, m, :], nzi[:, m:m + 1], bbr[:, m, :], ALU.mult, ALU.add, [bbr], [Bim, nzi, bbr])
                ts(C, "dve", bbi[:, m, :], Bim[:, m, :], zr[:, m:m + 1], None, ALU.mult, None, [bbi], [Bim, zr])
                stt(C, bbi[:, m, :], Bre[:, m, :], zi[:, m:m + 1], bbi[:, m, :], ALU.mult, ALU.add, [bbi], [Bre, zi, bbi])
            in3 = sb(C, st, [128, 32, 128], F32, "in3")
            K.op("pool", lambda e: e.memset(in3[:], 0.0), outs=[in3])
            for (bb, Bw) in ((bbr, Bw_re), (bbi, Bw_im)):
                for m in range(32):
                    for gl in range(2):
                        c0 = (2 * (m % 4) + gl) * 16
                        cp(C, "dve" if gl else "pool", in3[gl * 64:(gl + 1) * 64, m, c0:c0 + 16],
                           bb[gl * 64:(gl + 1) * 64, m, :], [in3], [bb])
                for m4 in range(8):
                    ps = C.PS[m4 % 2]
                    for j in range(4):
                        m = m4 * 4 + j
                        K.op("pe", lambda e: e.transpose(out=ps[:, j * 128:(j + 1) * 128], in_=in3[:, m, :],
                                                         identity=C.ident[:]), outs=[ps], ins=[in3, C.ident], inc=(j == 3))
                    cp(C, "act" if m4 % 2 else "dve", Bw[:, m4 * 4:(m4 + 1) * 4, :],
                       ps[:].rearrange("p (j n) -> p j n", j=4), [Bw], [ps])
            mask2 = sb(C, st, [128, 128], F32, "mask2")
            for a in range(4):
                K.op("pool", lambda e: e.affine_select(out=mask2[a * 32:(a + 1) * 32, 0:64], in_=C.ones_f[a * 32:(a + 1) * 32, 0:64],
                                                       pattern=[[0, 64]], compare_op=ALU.is_ge, fill=0.0, base=15,
                                                       channel_multiplier=-1), outs=[mask2], ins=[C.ones_f])
                K.op("pool", lambda e: e.affine_select(out=mask2[a * 32:(a + 1) * 32, 64:128], in_=C.ones_f[a * 32:(a + 1) * 32, 0:64],
                                                       pattern=[[0, 64]], compare_op=ALU.is_ge, fill=0.0, base=-16,
                                                       channel_multiplier=1), outs=[mask2], ins=[C.ones_f])
            Cre = sb(C, st, [128, 8, 64], F32, "Cre")
            Cim = sb(C, st, [128, 8, 64], F32, "Cim")
            K.dma("sp", Cre[:], P["l1_c_re"].rearrange("(ct gq) c p -> (gq c) ct p", gq=8), outs=[Cre])
            K.dma("sp", Cim[:], P["l1_c_im"].rearrange("(ct gq) c p -> (gq c) ct p", gq=8), outs=[Cim])
            in4 = sb(C, st, [128, 4, 128], F32, "in4")
            K.op("pool", lambda e: e.memset(in4[:], 0.0), outs=[in4])
            ni = 0
            for (Cc, Cw, sc) in ((Cre, Cw_re, 1.0), (Cim, Cw_in, -1.0)):
                for ct in range(8):
                    for j4 in range(4):
                        for gl in range(2):
                            tt(C, "dve" if gl else "pool", in4[j4 * 32:(j4 + 1) * 32, j4, gl * 64:(gl + 1) * 64],
                               Cc[j4 * 32:(j4 + 1) * 32, ct, :], mask2[j4 * 32:(j4 + 1) * 32, gl * 64:(gl + 1) * 64],
                               ALU.mult, [in4], [Cc, mask2])
                    ps = C.PS[2 + ni % 2]
                    ni += 1
                    for j4 in range(4):
                        K.op("pe", lambda e: e.transpose(out=ps[:, j4 * 128:(j4 + 1) * 128], in_=in4[:, j4, :],
                                                         identity=C.ident[:]), outs=[ps], ins=[in4, C.ident], inc=(j4 == 3))
                    act(C, Cw[:, ct * 4:(ct + 1) * 4, :], ps[:].rearrange("p (j n) -> p j n", j=4), AF.Copy, [Cw], [ps],
                        scale=sc)
            K.barrier()

        for s in range(2):
            with ExitStack() as sts:
                uTa = sb(C, sts, [128, 8, 2048], BF16, "uT")
                yga = sb(C, sts, [128, 8, 2048], BF16, "yg")
                uT = [Tile(uTa.ap) for _ in range(4)]
                yg = [Tile(yga.ap) for _ in range(4)]
                with ExitStack() as st:
                    w1 = sb(C, st, [128, 8, 1024], BF16, "w1in")
                    load_wbf(C, w1, P["l1_w_in"], 1024)
                    xt = [sb(C, st, [128, 1024], F32, "xt") for _ in range(3)]
                    xTb = [sb(C, st, [128, 8, 512], BF16, "xTb") for _ in range(2)]
                    nx = 0
                    for blk in range(4):
                        xb_ = xTb[blk % 2]
                        for t4 in range(4):
                            x = xt[nx % 3]
                            nx += 1
                            r0 = s * SEQ + blk * 512 + t4 * 128
                            K.dma("sp", x[:], src[r0:r0 + 128, :], outs=[x])
                            transpose8(C, x, xb_, lambda k0: xb_[:, k0:k0 + 4, t4 * 128:(t4 + 1) * 128], C.PS[0], C.PS[1])
                        for ft in range(8):
                            ps = C.PS[2 + ft % 4]
                            for kt in range(8):
                                mm(C, ps[:], w1[:, kt, ft * 128:(ft + 1) * 128], xb_[:, kt, :], kt == 0, kt == 7,
                                   [ps], [w1, xb_])
                            cp(C, "act" if ft % 2 else "dve", uTa[:, ft, blk * 512:(blk + 1) * 512], ps[:], [uT[blk]], [ps])
                    K.barrier()
                with ExitStack() as st:
                    cst = sb(C, st, [128, 4, 513], F32, "cst")
                    snt = sb(C, st, [128, 4, 513], F32, "snt")
                    nsl = sb(C, st, [128, 4], F32, "nsl")
                    rfull = sb(C, st, [128, 4, 512], F32, "rfull")
                    tht = sb(C, st, [128, 513], F32, "tht")
                    ta = sb(C, st, [128, 513], F32, "ta")
                    tb = sb(C, st, [128, 513], F32, "tb")
                    tki = sb(C, st, [128, 513], I32, "tki")
                    car = [sb(C, st, [128, 2], F32, "car") for _ in range(4)]
                    sets = []
                    for _k in range(3):
                        sets.append(dict(
                            br=sb(C, st, [128, 512], F32, "br"), bi=sb(C, st, [128, 512], F32, "bi"),
                            tq=[sb(C, st, [128, 512], F32, "tq") for _ in range(4)],
                            zin=None,
                            zz=[sb(C, st, [128, 512], F32, "zz") for _ in range(2)],
                            xr=sb(C, st, [128, 512], BF16, "xr"), xi=sb(C, st, [128, 512], BF16, "xi"),
                            cart=sb(C, st, [128, 2], F32, "cart")))
                    GT = GeluTmp(C, st, 512)
                    nit = 0
                    for ct in range(8):
                        for j4 in range(4):
                            m = ct * 4 + j4
                            ts(C, "dve", tht[:], iot[:], thr[:, m:m + 1], None, ALU.mult, None, [tht], [iot, thr])
                            sincos(C, tht[:], [tht], 513, snt[:, j4, :], [snt], cst[:, j4, :], [cst], ta, tb, tki)
                            ts(C, "dve", nsl[:, j4:j4 + 1], snt[:, j4, 512:513], -1.0, None, ALU.mult, None, [nsl], [snt])
                            ts(C, "pool", rfull[:, j4, :], iot[:, 0:512], 0.0, mag[:, m:m + 1], ALU.mult, ALU.add,
                               [rfull], [iot, mag])
                            K.op("pool", lambda e: e.memset(car[j4][:], 0.0), outs=[car[j4]])
                        its = [(c, j4) for c in range(4) for j4 in range(4)]

                        def setof(n):
                            S_ = sets[n % 3]
                            return (S_["br"], S_["bi"], S_["tq"], S_["zin"], S_["zz"], S_["xr"], S_["xi"], S_["cart"])

                        def stB(n, c, j4):
                            m = ct * 4 + j4
                            pr = C.PS[0]
                            pi_ = C.PS[1]
                            br, bi, tq, zin, zz, xr, xi, cart = setof(n)
                            rhs = uTa[:, ct, c * 512:(c + 1) * 512]
                            mm(C, pr[:], Bw_re[:, m, :], rhs, True, True, [pr], [Bw_re, uT[c]])
                            mm(C, pi_[:], Bw_im[:, m, :], rhs, True, True, [pi_], [Bw_im, uT[c]])
                            act(C, br[:], pr[:], AF.Copy, [br], [pr])
                            act(C, bi[:], pi_[:], AF.Copy, [bi], [pi_])

                        def stR1(n, c, j4):
                            br, bi, tq, zin, zz, xr, xi, cart = setof(n)
                            cs_ = cst[:, j4, 0:512]
                            sn_ = snt[:, j4, 0:512]
                            tt(C, "dve", tq[0][:], br[:], cs_, ALU.mult, [tq[0]], [br, cst])
                            tt(C, "pool", tq[1][:], bi[:], sn_, ALU.mult, [tq[1]], [bi, snt])
                            tt(C, "dve", tq[2][:], bi[:], cs_, ALU.mult, [tq[2]], [bi, cst])
                            tt(C, "pool", tq[3][:], br[:], sn_, ALU.mult, [tq[3]], [br, snt])
                            mm(C, C.PS[2][:], C.ident[:], tq[0][:], True, False, [C.PS[2]], [C.ident, tq[0]], inc=True)
                            mm(C, C.PS[2][:], C.ident[:], tq[1][:], False, True, [C.PS[2]], [C.ident, tq[1]], inc=True)
                            mm(C, C.PS[3][:], C.ident[:], tq[2][:], True, False, [C.PS[3]], [C.ident, tq[2]], inc=True)
                            mm(C, C.PS[3][:], C.nident[:], tq[3][:], False, True, [C.PS[3]], [C.nident, tq[3]], inc=True)

                        def stR2(n, c, j4):
                            br, bi, tq, zin, zz, xr, xi, cart = setof(n)
                            for k in range(2):
                                K.op("dve", lambda e: e.tensor_tensor_scan(out=zz[k][:], data0=rfull[:, j4, :],
                                                                           data1=C.PS[2 + k][:], initial=car[j4][:, k:k + 1],
                                                                           op0=ALU.mult, op1=ALU.add),
                                     outs=[zz[k]], ins=[rfull, C.PS[2 + k], car[j4]])
                            c5 = cst[:, j4, 512:513]
                            s5 = snt[:, j4, 512:513]
                            act(C, cart[:, 0:1], zz[0][:, 511:512], AF.Copy, [cart], [zz[0], cst], scale=c5)
                            act(C, cart[:, 1:2], zz[0][:, 511:512], AF.Copy, [cart], [zz[0], snt], scale=s5)
                            act(C, car[j4][:, 0:1], zz[1][:, 511:512], AF.Identity, [car[j4]], [zz[1], nsl, cart],
                                scale=nsl[:, j4:j4 + 1], bias=cart[:, 0:1])
                            act(C, car[j4][:, 1:2], zz[1][:, 511:512], AF.Identity, [car[j4]], [zz[1], cst, cart],
                                scale=c5, bias=cart[:, 1:2])

                        def stR3C(n, c, j4):
                            m = ct * 4 + j4
                            br, bi, tq, zin, zz, xr, xi, cart = setof(n)
                            cs_ = cst[:, j4, 0:512]
                            sn_ = snt[:, j4, 0:512]
                            yps = C.PS[4 + (ct * 4 + c) % 2]
                            tt(C, "dve", tq[0][:], zz[0][:], cs_, ALU.mult, [tq[0]], [zz[0], cst])
                            tt(C, "pool", tq[1][:], zz[1][:], sn_, ALU.mult, [tq[1]], [zz[1], snt])
                            tt(C, "dve", tq[3][:], zz[1][:], cs_, ALU.mult, [tq[3]], [zz[1], cst])
                            tt(C, "pool", tq[2][:], zz[0][:], sn_, ALU.mult, [tq[2]], [zz[0], snt])
                            mm(C, C.PS[6][:], C.ident[:], tq[0][:], True, False, [C.PS[6]], [C.ident, tq[0]], inc=True)
                            mm(C, C.PS[6][:], C.nident[:], tq[1][:], False, True, [C.PS[6]], [C.nident, tq[1]], inc=True)
                            act(C, xr[:], C.PS[6][:], AF.Copy, [xr], [C.PS[6]])
                            tt(C, "pool", xi[:], tq[2][:], tq[3][:], ALU.add, [xi], [tq[2], tq[3]])
                            mm(C, yps[:], Cw_re[:, m, :], xr[:], j4 == 0, False, [yps], [Cw_re, xr], inc=True)
                            mm(C, yps[:], Cw_in[:, m, :], xi[:], False, j4 == 3, [yps], [Cw_in, xi], inc=True)
                            if j4 == 3:
                                stt(C, GT.xs[:], uTa[:, ct, c * 512:(c + 1) * 512], dsk[:, ct:ct + 1], yps[:], ALU.mult,
                                    ALU.add, [GT.xs], [uT[c], dsk, yps])
                                gelu(C, None, None, yga[:, ct, c * 512:(c + 1) * 512], [yg[c]], GT, 512, xs_given=True)

                        stB(0, *its[0])
                        stR1(0, *its[0])
                        stB(1, *its[1])
                        for n, (c, j4) in enumerate(its):
                            if n + 2 < len(its):
                                stB(n + 2, *its[n + 2])
                            stR2(n, c, j4)
                            if n + 1 < len(its):
                                stR1(n + 1, *its[n + 1])
                            stR3C(n, c, j4)
                    K.barrier()
                with ExitStack() as st:
                    wv = sb(C, st, [128, 8, 1024], BF16, "wval")
                    wg = sb(C, st, [128, 8, 1024], BF16, "wgate")
                    load_wbf(C, wv, P["l1_w_val"], 1024)
                    load_wbf(C, wg, P["l1_w_gate"], 1024)
                    g1 = sb(C, st, [128, 1024], F32, "g1")
                    b1 = sb(C, st, [128, 1024], F32, "b1")
                    load_bc(C, g1, P["l1_ln1_g"])
                    load_bc(C, b1, P["l1_ln1_b"])
                    xt = [sb(C, st, [128, 1024], F32, "xt") for _ in range(3)]
                    sg = [sb(C, st, [128, 512], F32, "sgm") for _ in range(2)]
                    hv = [sb(C, st, [128, 512], F32, "hv") for _ in range(2)]
                    zs = [sb(C, st, [128, 1024], F32, "z") for _ in range(2)]
                    LTs = [LNTmp(C, st) for _ in range(2)]
                    ot = [sb(C, st, [128, 1024], F32, "ot") for _ in range(2)]

                    def ostage1(t16):
                        x = xt[t16 % 3]
                        z = zs[t16 % 2]
                        r0 = s * SEQ + t16 * 128
                        K.dma("sp", x[:], src[r0:r0 + 128, :], outs=[x])
                        for half in range(2):
                            pv = C.PS[(t16 % 2) * 4 + half]
                            pg = C.PS[(t16 % 2) * 4 + 2 + half]
                            for ft in range(8):
                                mm(C, pv[:], yga[:, ft, t16 * 128:(t16 + 1) * 128], wv[:, ft, half * 512:(half + 1) * 512],
                                   ft == 0, ft == 7, [pv], [yg[t16 // 4], wv])
                            for ft in range(8):
                                mm(C, pg[:], yga[:, ft, t16 * 128:(t16 + 1) * 128], wg[:, ft, half * 512:(half + 1) * 512],
                                   ft == 0, ft == 7, [pg], [yg[t16 // 4], wg])
                            act(C, sg[half][:], pg[:], AF.Sigmoid, [sg[half]], [pg])
                            tt(C, "dve", hv[half][:], pv[:], sg[half][:], ALU.mult, [hv[half]], [pv, sg[half]])
                            stt(C, z[:, half * 512:(half + 1) * 512], x[:, half * 512:(half + 1) * 512], ALPHA, hv[half][:],
                                ALU.mult, ALU.add, [z], [x, hv[half]])

                    def ostage2(t16):
                        r0 = s * SEQ + t16 * 128
                        o = ot[t16 % 2]
                        layernorm(C, zs[t16 % 2], o, LTs[t16 % 2], g1, b1)
                        K.dma("sp", dst[r0:r0 + 128, :], o[:], ins=[o])

                    ostage1(0)
                    for t16 in range(16):
                        if t16 + 1 < 16:
                            ostage1(t16 + 1)
                        ostage2(t16)
                    K.barrier()


def build(stop_after=None):
    nc = bass.Bass("TRN2", target_bir_lowering=False)
    C = Ctx()
    C.nc = nc
    C.n = 0
    P = {}
    P["x"] = nc.dram_tensor("x", [NTOK, D], F32, kind="ExternalInput").ap()
    P["mem"] = nc.dram_tensor("mem", [512, D], F32, kind="ExternalInput").ap()
    for k, shp in PARAM_SHAPES.items():
        P[k] = nc.dram_tensor(k, list(shp), F32, kind="ExternalInput").ap()
    out = nc.dram_tensor("out", [NTOK, D], F32, kind="ExternalOutput").ap()
    xa = nc.dram_tensor("xa", [NTOK, D], F32, kind="Internal").ap()
    xb = nc.dram_tensor("xb", [NTOK, D], F32, kind="Internal").ap()
    xc = nc.dram_tensor("xc", [NTOK, D], F32, kind="Internal").ap()
    Xs = nc.dram_tensor("Xs", [NBLK * BSZ, D], BF16, kind="Internal").ap()
    Ys = nc.dram_tensor("Ys", [NBLK * BSZ, D], BF16, kind="Internal").ap()
    C.P = P
    es = ExitStack()
    with es:
        C.es = es
        C.K = Sched(nc, es)
        C.PS = [Tile(es.enter_context(nc.psum_tensor("ps%d" % i, [128, 512], F32))) for i in range(8)]
        phase_consts(C)
        phase_kv(C)

        def final(src_ap):
            if src_ap is not out:
                C.K.dma("sp", out, src_ap)
            C.K.barrier()

        for s in range(2):
            with ExitStack() as st:
                dT = sb(C, st, [128, 4, 2048], BF16, "dT")
                phase_l0_attn(C, s, dT, P["x"])
                phase_l0_sgu_out(C, s, dT, P["x"], xa)
        if stop_after == "l0mix":
            final(xa)
            return nc
        phase_cross(C, "l0_", xa, xb)
        if stop_after == "l0cross":
            final(xb)
            return nc
        phase_moe_sparse(C, "l0_", xb, xc, Xs, Ys)
        if stop_after == "l0":
            final(xc)
            return nc
        phase_s5(C, xc, xa)
        if stop_after == "l1mix":
            final(xa)
            return nc
        phase_cross(C, "l1_", xa, xb)
        phase_moe_sparse(C, "l1_", xb, out, Xs, Ys)
    return nc


def make_in_maps(inputs):
    x = np.ascontiguousarray(inputs["x"], dtype=np.float32)
    mem = np.ascontiguousarray(inputs["mem"], dtype=np.float32)
    shared = {k: np.ascontiguousarray(inputs[k], dtype=np.float32) for k in PARAM_SHAPES}
    maps = []
    for c in range(NCORES):
        m = dict(shared)
        m["x"] = x[2 * c:2 * c + 2].reshape(NTOK, D)
        m["mem"] = mem[2 * c:2 * c + 2].reshape(512, D)
        maps.append(m)
    return maps


def kernel(**inputs):
    nc = build()
    maps = make_in_maps(inputs)
    res = run_bass_kernel_spmd(nc, maps, core_ids=list(range(NCORES)))
    outs = [np.asarray(r["out"]).reshape(2, SEQ, D) for r in res.results]
    return np.concatenate(outs, axis=0).astype(np.float32)
```
